# Optimizing a Trainium2 kernel written in Bass

```python
import math, functools
import jax, jax.numpy as jnp
from jax import lax
import numpy as np

D_MODEL = 2048
BATCH = 2
SEQ = 4096
DEPTH = 1
DEC_BATCH = 128
DEC_SEQ = 4
PAST_LEN = 2048
PAGE_SIZE = 128

N_META = 16
N_HEADS = 16
HEAD_DIM = 128
N_KV_HEADS = 4
GROUP = N_HEADS // N_KV_HEADS
D_ATTN = N_HEADS * HEAD_DIM
D_KV = N_KV_HEADS * HEAD_DIM
N_IDX_HEADS = 16
IDX_DIM = 64
TOPK_MAX = 256
D_CONV = D_MODEL
CONV_W = 3
D_FF = 5632
Q_BLOCK = 128
LN_EPS = 1e-5
ALPHA = (2.0 * DEPTH) ** 0.25
BETA = (8.0 * DEPTH) ** -0.25
PROJ_SIZES = (D_CONV, D_CONV, D_CONV, D_ATTN, D_KV, D_KV, N_IDX_HEADS * IDX_DIM, IDX_DIM, N_IDX_HEADS, D_MODEL, D_MODEL)
V_COL_INDEX = 5
D_IN = sum(PROJ_SIZES)

kernel_name = "hybrid_shortconv_dsa_macaron_deepnorm_step"


def layer_norm(x, g, b):
    xf = x.astype(jnp.float32)
    mu = jnp.mean(xf, axis=-1, keepdims=True)
    var = jnp.mean(jnp.square(xf - mu), axis=-1, keepdims=True)
    y = (xf - mu) * lax.rsqrt(var + LN_EPS) * g.astype(jnp.float32) + b.astype(jnp.float32)
    return y.astype(x.dtype)


def swiglu(x, w_gu, w_down):
    g, u = jnp.split(x @ w_gu, 2, axis=-1)
    return (jax.nn.silu(g) * u) @ w_down


def split_proj(p):
    offs = tuple(int(o) for o in np.cumsum(PROJ_SIZES)[:-1])
    return jnp.split(p, offs, axis=-1)


def alibi_slopes():
    h = jnp.arange(1, N_HEADS + 1, dtype=jnp.float32)
    return jnp.exp2(-8.0 * h / N_HEADS).reshape(N_KV_HEADS, GROUP)


def short_conv(u, prev, w):
    L = u.shape[1]
    u_pad = jnp.concatenate([prev, u], axis=1)
    y = sum(w[j] * u_pad[:, j:j + L] for j in range(CONV_W))
    return y, u_pad[:, -(CONV_W - 1):]


def indexer_scores(iq, iw, ik, qpos):
    s = jax.nn.relu(jnp.einsum('bqhd,bsd->bqhs', iq, ik))
    sc = jnp.einsum('bqhs,bqh->bqs', s, iw).astype(jnp.float32)
    kpos = jnp.arange(ik.shape[1])
    return jnp.where(kpos[None, None, :] <= qpos[None, :, None], sc, -jnp.inf)


def sparse_attend(q, k_sel, v_sel, sel, qpos):
    B, Q = q.shape[:2]
    qg = q.reshape(B, Q, N_KV_HEADS, GROUP, HEAD_DIM)
    s = jnp.einsum('bqhgd,bqnhd->bqhgn', qg, k_sel).astype(jnp.float32) * (HEAD_DIM ** -0.5)
    dist = qpos[None, :, None] - sel
    s = s - alibi_slopes()[None, None, :, :, None] * dist.astype(jnp.float32)[:, :, None, None, :]
    s = jnp.where((dist >= 0)[:, :, None, None, :], s, -jnp.inf)
    p = jax.nn.softmax(s, axis=-1).astype(v_sel.dtype)
    o = jnp.einsum('bqhgn,bqnhd->bqhgd', p, v_sel)
    return o.reshape(B, Q, D_ATTN)


gather_rows = jax.vmap(lambda a, i: a[i])


def prompt_attention(q, k, v, iq, iw, ik):
    B, T = q.shape[:2]
    n_sel = min(TOPK_MAX, T // 4)
    nb = -(-T // Q_BLOCK)
    T_pad = nb * Q_BLOCK
    pad = lambda a: jnp.pad(a, [(0, 0), (0, T_pad - T)] + [(0, 0)] * (a.ndim - 2))
    qp, iqp, iwp = pad(q), pad(iq), pad(iw)

    def block(i):
        start = i * Q_BLOCK
        qb = lax.dynamic_slice_in_dim(qp, start, Q_BLOCK, axis=1)
        iqb = lax.dynamic_slice_in_dim(iqp, start, Q_BLOCK, axis=1)
        iwb = lax.dynamic_slice_in_dim(iwp, start, Q_BLOCK, axis=1)
        qpos = start + jnp.arange(Q_BLOCK)
        sc = indexer_scores(iqb, iwb, ik, qpos)
        _, sel = lax.top_k(sc, n_sel)
        return sparse_attend(qb, gather_rows(k, sel), gather_rows(v, sel), sel, qpos)

    out = lax.map(block, jnp.arange(nb))
    return jnp.moveaxis(out, 0, 1).reshape(B, T_pad, D_ATTN)[:, :T]


def sample_attention(q, k, v, iq, iw, ik, cache_k, cache_v, cache_ik, page_table):
    DB, S = q.shape[:2]
    past = page_table.shape[1] * PAGE_SIZE
    n_sel = min(TOPK_MAX, (past + S) // 4)
    ik_past = cache_ik[page_table].reshape(DB, past, IDX_DIM)
    ik_all = jnp.concatenate([ik_past, ik], axis=1)
    qpos = past + jnp.arange(S)
    sc = indexer_scores(iq, iw, ik_all, qpos)
    _, sel = lax.top_k(sc, n_sel)
    in_past = (sel < past)[..., None, None]
    sp = jnp.minimum(sel, past - 1)
    phys = page_table[jnp.arange(DB)[:, None, None], sp // PAGE_SIZE]
    off = sp % PAGE_SIZE
    sn = jnp.clip(sel - past, 0, S - 1)
    k_sel = jnp.where(in_past, cache_k[phys, off], gather_rows(k, sn))
    v_sel = jnp.where(in_past, cache_v[phys, off], gather_rows(v, sn))
    return sparse_attend(q, k_sel, v_sel, sel, qpos)


def token_mix(x, conv_prev, attn_fn, lp):
    B, T = x.shape[:2]
    gb, gc, h, q, k, v, iq, ik, iw, g_conv, g_attn = split_proj(x @ lp['w_in'])
    cv, conv_state = short_conv(gc * h, conv_prev, lp['conv_w'])
    y_conv = (gb * cv) @ lp['w_conv_out']
    q = q.reshape(B, T, N_HEADS, HEAD_DIM)
    k = k.reshape(B, T, N_KV_HEADS, HEAD_DIM)
    v = v.reshape(B, T, N_KV_HEADS, HEAD_DIM)
    iq = iq.reshape(B, T, N_IDX_HEADS, IDX_DIM)
    iw = iw * ((N_IDX_HEADS * IDX_DIM) ** -0.5)
    y_attn = attn_fn(q, k, v, iq, iw, ik) @ lp['w_attn_out']
    merged = jax.nn.sigmoid(g_conv) * y_conv + jax.nn.sigmoid(g_attn) * y_attn
    return merged @ lp['w_o'], (k, v, ik, conv_state)


def layer(x, conv_prev, attn_fn, lp):
    x = layer_norm(ALPHA * x + 0.5 * swiglu(x, lp['ffn1_w_gu'], lp['ffn1_w_down']), lp['ln1_g'], lp['ln1_b'])
    mix, state = token_mix(x, conv_prev, attn_fn, lp)
    x = layer_norm(ALPHA * x + mix, lp['ln2_g'], lp['ln2_b'])
    x = layer_norm(ALPHA * x + 0.5 * swiglu(x, lp['ffn2_w_gu'], lp['ffn2_w_down']), lp['ln3_g'], lp['ln3_b'])
    return x, state


def setup_inputs(seed: int = 0) -> dict:
    key = jax.random.key(seed)
    ks = jax.random.split(key, 24)
    nrm = lambda k, shape: jax.random.normal(k, shape, jnp.float32)
    n_pages = PAST_LEN // PAGE_SIZE
    n_pool = (DEC_BATCH * n_pages * 5) // 4
    col_scale = jnp.concatenate([jnp.full((s,), BETA if i == V_COL_INDEX else 1.0, jnp.float32) for i, s in enumerate(PROJ_SIZES)])
    return {
        'x_prompt': nrm(ks[0], (BATCH, SEQ, D_MODEL)),
        'x_sample': nrm(ks[1], (DEC_BATCH, DEC_SEQ, D_MODEL)),
        'cache_k': nrm(ks[2], (DEPTH, n_pool, PAGE_SIZE, N_KV_HEADS, HEAD_DIM)),
        'cache_v': BETA * nrm(ks[3], (DEPTH, n_pool, PAGE_SIZE, N_KV_HEADS, HEAD_DIM)),
        'cache_idx_k': nrm(ks[4], (DEPTH, n_pool, PAGE_SIZE, IDX_DIM)),
        'state_conv': nrm(ks[5], (DEPTH, DEC_BATCH, CONV_W - 1, D_CONV)),
        'page_table': jax.random.permutation(ks[6], n_pool)[:DEC_BATCH * n_pages].reshape(DEC_BATCH, n_pages).astype(jnp.int32),
        'meta_tokens': nrm(ks[7], (N_META, D_MODEL)),
        'w_in': nrm(ks[8], (DEPTH, D_MODEL, D_IN)) * (D_MODEL ** -0.5) * col_scale,
        'conv_w': nrm(ks[9], (DEPTH, CONV_W, D_CONV)) * (CONV_W ** -0.5),
        'w_conv_out': nrm(ks[10], (DEPTH, D_CONV, D_MODEL)) * (D_CONV ** -0.5),
        'w_attn_out': nrm(ks[11], (DEPTH, D_ATTN, D_MODEL)) * (D_ATTN ** -0.5),
        'w_o': nrm(ks[12], (DEPTH, D_MODEL, D_MODEL)) * (D_MODEL ** -0.5) * BETA,
        'ffn1_w_gu': nrm(ks[13], (DEPTH, D_MODEL, 2 * D_FF)) * (D_MODEL ** -0.5),
        'ffn1_w_down': nrm(ks[14], (DEPTH, D_FF, D_MODEL)) * (D_FF ** -0.5) * BETA,
        'ffn2_w_gu': nrm(ks[15], (DEPTH, D_MODEL, 2 * D_FF)) * (D_MODEL ** -0.5),
        'ffn2_w_down': nrm(ks[16], (DEPTH, D_FF, D_MODEL)) * (D_FF ** -0.5) * BETA,
        'ln1_g': 1.0 + 0.02 * nrm(ks[17], (DEPTH, D_MODEL)),
        'ln1_b': 0.02 * nrm(ks[18], (DEPTH, D_MODEL)),
        'ln2_g': 1.0 + 0.02 * nrm(ks[19], (DEPTH, D_MODEL)),
        'ln2_b': 0.02 * nrm(ks[20], (DEPTH, D_MODEL)),
        'ln3_g': 1.0 + 0.02 * nrm(ks[21], (DEPTH, D_MODEL)),
        'ln3_b': 0.02 * nrm(ks[22], (DEPTH, D_MODEL)),
    }


def reference(x_prompt, x_sample, cache_k, cache_v, cache_idx_k, state_conv, page_table, meta_tokens,
              w_in, conv_w, w_conv_out, w_attn_out, w_o, ffn1_w_gu, ffn1_w_down, ffn2_w_gu, ffn2_w_down,
              ln1_g, ln1_b, ln2_g, ln2_b, ln3_g, ln3_b):
    B = x_prompt.shape[0]
    meta = jnp.broadcast_to(meta_tokens[None].astype(x_prompt.dtype), (B, N_META, D_MODEL))
    hp = jnp.concatenate([meta, x_prompt], axis=1)
    hs = x_sample
    kp_l, vp_l, ikp_l, cp_l, ks_l, vs_l, iks_l, cs_l = [], [], [], [], [], [], [], []
    for l in range(DEPTH):
        lp = {'w_in': w_in[l], 'conv_w': conv_w[l], 'w_conv_out': w_conv_out[l], 'w_attn_out': w_attn_out[l],
              'w_o': w_o[l], 'ffn1_w_gu': ffn1_w_gu[l], 'ffn1_w_down': ffn1_w_down[l],
              'ffn2_w_gu': ffn2_w_gu[l], 'ffn2_w_down': ffn2_w_down[l],
              'ln1_g': ln1_g[l], 'ln1_b': ln1_b[l], 'ln2_g': ln2_g[l], 'ln2_b': ln2_b[l],
              'ln3_g': ln3_g[l], 'ln3_b': ln3_b[l]}
        conv_zero = jnp.zeros((B, CONV_W - 1, D_CONV), hp.dtype)
        hp, (kp, vp, ikp, cp) = layer(hp, conv_zero, prompt_attention, lp)
        sample_fn = functools.partial(sample_attention, cache_k=cache_k[l], cache_v=cache_v[l],
                                      cache_ik=cache_idx_k[l], page_table=page_table)
        hs, (ks, vs, iks, cs) = layer(hs, state_conv[l], sample_fn, lp)
        kp_l.append(kp); vp_l.append(vp); ikp_l.append(ikp); cp_l.append(cp)
        ks_l.append(ks); vs_l.append(vs); iks_l.append(iks); cs_l.append(cs)
    y_prompt = hp[:, N_META:]
    y_sample = hs
    return (y_prompt, y_sample,
            jnp.stack(kp_l), jnp.stack(vp_l), jnp.stack(ikp_l), jnp.stack(cp_l),
            jnp.stack(ks_l), jnp.stack(vs_l), jnp.stack(iks_l), jnp.stack(cs_l))
```

```python
import numpy as np
from contextlib import ExitStack
import concourse.bass as bass
import concourse.mybir as mybir
from concourse.bass_utils import run_bass_kernel_spmd

F32, BF16, I32 = mybir.dt.float32, mybir.dt.bfloat16, mybir.dt.int32
ALU = mybir.AluOpType
AF = mybir.ActivationFunctionType
AX = mybir.AxisListType


class Tok:
    __slots__ = ("w", "r", "name")

    def __init__(self, name=""):
        self.w = None
        self.r = {}
        self.name = name


class Eng:
    def __init__(self, K, name, same_raw):
        self.K, self.name, self.same_raw = K, name, same_raw
        self.sem = K.new_sem(name)
        self.cnt = 0
        self.waited = {}
        self.prog = []


class Kern:
    EPOCH = 30000

    def __init__(self, nc, es):
        self.nc, self.es = nc, es
        self.nsem = 0
        self.PE = Eng(self, "pe", False)
        self.ACT = Eng(self, "act", True)
        self.DVE = Eng(self, "dve", True)
        self.POOL = Eng(self, "pool", True)
        self.SP = Eng(self, "sp", True)
        self.rings = {}
        for q, n in (("sp", 20), ("pool", 6)):
            self.rings[q] = dict(sems=[self.new_sem(f"d{q}{i}") for i in range(n)], vals=[0] * n, k=0)
        self.out_events = []

    def new_sem(self, name):
        self.nsem += 1
        return self.es.enter_context(self.nc.semaphore(f"{name}_{self.nsem}"))

    def _waits(self, eng, rd, wr):
        deps = []
        for t in rd:
            if t.w is not None:
                deps.append(t.w)
        for t in wr:
            if t.w is not None:
                deps.append(t.w)
            deps.extend(t.r.values())
        for (sem, val, en) in deps:
            if en == eng.name and not eng.same_raw:
                continue
            key = id(sem)
            if eng.waited.get(key, 0) >= val:
                continue
            eng.waited[key] = val
            eng.prog.append(lambda e, sem=sem, val=val: e.wait_ge(sem, val))

    def _mark(self, ev, rd, wr):
        for t in rd:
            t.r[id(ev[0])] = ev
        for t in wr:
            t.w = ev
            t.r = {}

    def op(self, eng, fn, rd=(), wr=(), inc=True):
        self._waits(eng, rd, wr)
        if eng.cnt >= self.EPOCH and inc:
            pass
        ev = (eng.sem, eng.cnt + 1, eng.name)
        if inc:
            sem = eng.sem
            eng.prog.append(lambda e, fn=fn, sem=sem: fn(e).then_inc(sem, 1))
            eng.cnt += 1
            if eng.cnt >= self.EPOCH:
                eng.sem = self.new_sem(eng.name)
                eng.cnt = 0
        else:
            eng.prog.append(lambda e, fn=fn: fn(e))
        self._mark(ev, rd, wr)
        return ev

    def dma(self, eng, out, in_, rd=(), wr=(), fn=None, is_out=False):
        ring = self.rings[eng.name]
        i = ring["k"] % len(ring["sems"])
        ring["k"] += 1
        sem, prev = ring["sems"][i], ring["vals"][i]
        self._waits(eng, rd, wr)
        key = id(sem)
        if prev > 0 and eng.waited.get(key, 0) < prev:
            eng.waited[key] = prev
            eng.prog.append(lambda e, sem=sem, prev=prev: e.wait_ge(sem, prev))
        if fn is None:
            fn = lambda e, out=out, in_=in_: e.dma_start(out=out, in_=in_)
        eng.prog.append(lambda e, fn=fn, sem=sem: fn(e).then_inc(sem, 16))
        ring["vals"][i] = prev + 16
        ev = (sem, prev + 16, "dma")
        self._mark(ev, rd, wr)
        if is_out:
            self.out_events.append(ev)
        return ev

    def wait_event(self, eng, ev):
        sem, val, _ = ev
        if eng.waited.get(id(sem), 0) < val:
            eng.waited[id(sem)] = val
            eng.prog.append(lambda e, sem=sem, val=val: e.wait_ge(sem, val))

    def mm(self, out, lhsT, rhs, start, stop, rd, wr):
        return self.op(self.PE, lambda e: e.matmul(out, lhsT, rhs, start=start, stop=stop), rd, wr, inc=stop)

    def tr(self, out, in_, ident, rd, wr, inc=True):
        return self.op(self.PE, lambda e: e.transpose(out, in_, ident), rd, wr, inc=inc)

    def act(self, out, in_, func, rd, wr, bias=None, scale=None, accum_out=None, eng=None):
        kw = {}
        if bias is not None:
            kw["bias"] = bias
        if scale is not None:
            kw["scale"] = scale
        if accum_out is not None:
            kw["accum_out"] = accum_out
        return self.op(eng or self.ACT, lambda e: e.activation(out=out, in_=in_, func=func, **kw), rd, wr)

    def tt(self, out, in0, in1, op, rd, wr, eng=None):
        return self.op(eng or self.DVE, lambda e: e.tensor_tensor(out=out, in0=in0, in1=in1, op=op), rd, wr)

    def ts(self, out, in0, s1, s2, op0, op1=None, rd=(), wr=(), accum_out=None, eng=None):
        kw = {}
        if op1 is not None:
            kw["op1"] = op1
        if accum_out is not None:
            kw["accum_out"] = accum_out
        return self.op(eng or self.DVE,
                       lambda e: e.tensor_scalar(out=out, in0=in0, scalar1=s1, scalar2=s2, op0=op0, **kw), rd, wr)

    def stt(self, out, in0, scalar, in1, op0, op1, rd, wr, eng=None):
        return self.op(eng or self.DVE,
                       lambda e: e.scalar_tensor_tensor(out=out, in0=in0, scalar=scalar, in1=in1, op0=op0, op1=op1),
                       rd, wr)

    def copy(self, out, in_, rd, wr, eng=None):
        return self.op(eng or self.DVE, lambda e: e.tensor_copy(out=out, in_=in_), rd, wr)

    def red(self, out, in_, op, rd, wr, eng=None):
        return self.op(eng or self.DVE, lambda e: e.tensor_reduce(out=out, in_=in_, axis=AX.X, op=op), rd, wr)

    def memset(self, ap, val, wr, eng=None):
        return self.op(eng or self.DVE, lambda e: e.memset(ap, val), (), wr)

    def finish(self):
        nc = self.nc
        for ev in self.out_events:
            self.wait_event(self.SP, ev)
        with nc.Block() as block:
            @block.tensor
            def _(e):
                for th in self.PE.prog:
                    th(e)

            @block.scalar
            def _(e):
                for th in self.ACT.prog:
                    th(e)

            @block.vector
            def _(e):
                for th in self.DVE.prog:
                    th(e)

            @block.gpsimd
            def _(e):
                for th in self.POOL.prog:
                    th(e)

            @block.sync
            def _(e):
                for th in self.SP.prog:
                    th(e)

    def coll(self, kind, ins, outs, groups, rd, wr):
        eng = self.POOL
        self._waits(eng, rd, wr)
        sem = self.new_sem("cc")
        eng.prog.append(lambda e: e.collective_compute(kind, ALU.bypass, replica_groups=groups,
                                                       ins=ins, outs=outs).then_inc(sem))
        ev = (sem, 1, "coll")
        self._mark(ev, rd, wr)
        return ev


D = 2048
T = 1152
NT = 9
TG = [(0, 512), (512, 512), (1024, 128)]
DFF = 5632
NU = 22
ALPHA = 2.0 ** 0.25
EPS = 1e-5
C0 = 25.0
SCALE = 128.0 ** -0.5
NITER = 20
import os
NPOOL = int(os.environ.get('KNPOOL', '2560'))
KSTOP = os.environ.get('KSTOP', '')
KCORES = int(os.environ.get('KCORES', '8'))


class _Stop(Exception):
    pass
FM_GB, FM_GC, FM_H, FM_Q, FM_KT, FM_IQ, FM_IK, FM_GCONV, FM_GATTN, NFM = 0, 16, 32, 48, 64, 68, 76, 77, 93, 109
NENT = 57


def build_program():
    nc = bass.Bass("TRN2", target_bir_lowering=False)
    es = ExitStack()
    din = lambda n, s, d=F32: nc.dram_tensor(n, list(s), d, kind="ExternalInput").ap()
    dout = lambda n, s, d=F32: nc.dram_tensor(n, list(s), d, kind="ExternalOutput").ap()
    dscr = lambda n, s, d: nc.dram_tensor(n, list(s), d)
    xin = din("xin", [NT, 128, D]); xTin = din("xTin", [128, 16, T])
    wgu = [din("wgu1", [NU, 128, 16, 2, 256]), din("wgu2", [NU, 128, 16, 2, 256])]
    wdn = [din("wd1", [NU, 128, 2, D]), din("wd2", [NU, 128, 2, D])]
    wfm = din("wfm", [NFM, 128, 16, 128])
    wk_d = din("wk", [128, 16, 512]); wv_d = din("wv", [128, 16, 512]); wikw_d = din("wikw", [128, 16, 80])
    wco = din("wco", [16, 128, 16, 128]); wao = din("wao", [16, 128, 16, 128]); wo_d = din("wo", [8, 128, 16, 256])
    lnp = din("lnp", [6, D]); convw_d = din("convw", [128, 16, 3]); sconv_d = din("sconv", [128, 16, 16, 2])
    ck_d = din("ck", [NPOOL * 128, 512]); cv_d = din("cv", [NPOOL * 128, 512]); cik_d = din("cik", [NPOOL * 128, 64])
    ptab_d = din("ptab", [1, 256], I32); iota_d = din("iota", [128, 1], I32)
    ident_d = din("ident", [128, 128]); cmask_d = din("cmask", [128, 512]); cms_d = din("cms", [4, 4])
    sel_d = din("sel", [64, 16]); cms64_d = din("cms64", [64, 4])
    alibl_d = din("alibl", [5, NENT, 128]); alibr_d = din("alibr", [4, 2048]); pow2_d = din("pow2", [1, NITER + 1])
    ms_d = din("ms", [1, 16]); t0_d = din("t0tab", [1, 9]); kpos_d = din("kpos", [1, 4112]); idt16_d = din("idt16", [128, 16, 128])
    y_blk = dout("y_blk", [8, 128, D]); y_misc = dout("y_misc", [128, D])
    k_blk = dout("k_blk", [8, 128, 512]); k_misc = dout("k_misc", [128, 512])
    v_blk = dout("v_blk", [8, 128, 512]); v_misc = dout("v_misc", [128, 512])
    ik_blk = dout("ik_blk", [8, 128, 64]); ik_misc = dout("ik_misc", [128, 64])
    ust_o = dout("ustate", [34, D])
    X1s = dscr("X1s", [NT, 128, D], F32); XTs = dscr("XTs", [128, 16, T], BF16)
    qTs = dscr("qTs", [16, 128, T], BF16); iqTs = dscr("iqTs", [8, 128, T], BF16)
    mcTs = dscr("mcTs", [16, 128, T], BF16); oTs = dscr("oTs", [16, 128, T], BF16)
    agki = dscr("agki", [512, 1024], BF16); agko = dscr("agko", [2048, 1024], BF16)
    agvi = dscr("agvi", [1024, 512], BF16); agvo = dscr("agvo", [4096, 512], BF16)
    agii = dscr("agii", [128, 1024], BF16); agio = dscr("agio", [512, 1024], BF16)
    vns = dscr("vns", [128, 512], BF16)
    sb = lambda n, s, d: es.enter_context(nc.sbuf_tensor(n, list(s), d))
    try:
        with es:
            K = Kern(nc, es)
            PE, ACTE, DVE, POOL, SP = K.PE, K.ACT, K.DVE, K.POOL, K.SP

            def stop(tag):
                if KSTOP == tag:
                    print("STOP", tag, {e.name: (e.cnt, len(e.prog)) for e in (PE, ACTE, DVE, POOL, SP)}, flush=True)
                    K.finish()
                    raise _Stop()
            FB = [es.enter_context(nc.psum_tensor(f"fb{i}", [128, 512], F32)) for i in range(6)]
            tFB = [Tok() for _ in range(6)]
            BB = [es.enter_context(nc.psum_tensor(f"bb{i}", [128, 1024], BF16)) for i in range(2)]
            tBB = [Tok() for _ in range(2)]
            st = dict(fa=0, bb=0)

            def fbank(lo=0, hi=6):
                i = lo + st["fa"] % (hi - lo)
                st["fa"] += 1
                return FB[i], tFB[i]

            def bbank():
                i = st["bb"] % 2
                st["bb"] += 1
                return BB[i], tBB[i]

            IDb = sb("IDb", [128, 128], BF16); tIDb = Tok()
            ONESb = sb("ONESb", [128, 128], BF16); tONES = Tok()
            STAT = sb("STAT", [128, 8, NT], F32); tSTAT = Tok()
            WQ = sb("WQ", [128, NT, 16], F32); tWQ = Tok()
            K.dma(POOL, IDb[:], ident_d[:, :], wr=[tIDb])
            K.memset(ONESb[:], 1.0, wr=[tONES])
            tXs, tXTs, tqTs, tiqTs, tmcTs, toTs = Tok(), Tok(), Tok(), Tok(), Tok(), Tok()

            def ffn(X, tX, XT, tXT, fi, scope):
                WGU = [scope("WGU0", [128, 16, 2, 256], BF16), scope("WGU1", [128, 16, 2, 256], BF16)]
                tWGU = [Tok(), Tok()]
                WD = scope("WD", [128, 2, D], BF16); tWD = Tok()
                HT = scope("HT", [128, 2, T], BF16); tHT = [[Tok() for _ in TG] for _ in range(2)]
                SG = [scope("SG0", [128, 512], F32), scope("SG1", [128, 512], F32)]; tSG = [Tok(), Tok()]
                n = 0
                for u in range(NU):
                    wg, tw = WGU[u % 2], tWGU[u % 2]
                    K.dma(POOL, wg[:], wgu[fi][u], wr=[tw])
                    K.dma(POOL, WD[:], wdn[fi][u], wr=[tWD])
                    for fc in range(2):
                        for gi, (g0, gn) in enumerate(TG):
                            pg, tpg = fbank(0, 4)
                            for kc in range(16):
                                K.mm(pg[:, :gn], wg[:, kc, 0, fc * 128:(fc + 1) * 128], XT[:, kc, g0:g0 + gn],
                                     kc == 0, kc == 15, [tw, tXT], [tpg])
                            pu, tpu = fbank(0, 4)
                            for kc in range(16):
                                K.mm(pu[:, :gn], wg[:, kc, 1, fc * 128:(fc + 1) * 128], XT[:, kc, g0:g0 + gn],
                                     kc == 0, kc == 15, [tw, tXT], [tpu])
                            sg, tsg = SG[n % 2], tSG[n % 2]
                            n += 1
                            K.act(sg[:, :gn], pg[:, :gn], AF.Silu, [tpg], [tsg])
                            K.tt(HT[:, fc, g0:g0 + gn], sg[:, :gn], pu[:, :gn], ALU.mult, [tsg, tpu], [tHT[fc][gi]])
                    for t in range(NT):
                        gi = min(t // 4, 2)
                        for dq in range(4):
                            po, tpo = fbank(4, 6)
                            for fc in range(2):
                                K.mm(po[:, :], HT[:, fc, t * 128:(t + 1) * 128], WD[:, fc, dq * 512:(dq + 1) * 512],
                                     fc == 0, fc == 1, [tHT[fc][gi], tWD], [tpo])
                            xs = X[:, t, dq * 512:(dq + 1) * 512]
                            K.stt(xs, po[:, :], 0.5, xs, ALU.mult, ALU.add, [tpo, tX[t][dq]], [tX[t][dq]])

            def ln_tiles(X, tX, li, scope):
                SQ = scope("SQ", [128, D], F32); tSQ = Tok()
                LNG = scope("LNG", [128, D], F32); LNB = scope("LNB", [128, D], F32); tLNG, tLNB = Tok(), Tok()
                K.dma(SP, LNG[:], lnp[2 * li:2 * li + 1, :].partition_broadcast(128), wr=[tLNG])
                K.dma(SP, LNB[:], lnp[2 * li + 1:2 * li + 2, :].partition_broadcast(128), wr=[tLNB])
                for t in range(NT):
                    K.red(STAT[:, 0, t:t + 1], X[:, t, :], ALU.add, tX[t], [tSTAT])
                    K.act(SQ[:], X[:, t, :], AF.Square, tX[t], [tSQ])
                    K.red(STAT[:, 1, t:t + 1], SQ[:], ALU.add, [tSQ], [tSTAT])
                S = lambda r: STAT[:, r, :]
                K.ts(S(2), S(0), 1.0 / D, None, ALU.mult, rd=[tSTAT], wr=[tSTAT])
                K.ts(S(3), S(1), 1.0 / D, None, ALU.mult, rd=[tSTAT], wr=[tSTAT])
                K.tt(S(4), S(2), S(2), ALU.mult, [tSTAT], [tSTAT])
                K.tt(S(3), S(3), S(4), ALU.subtract, [tSTAT], [tSTAT])
                K.ts(S(3), S(3), EPS, None, ALU.add, rd=[tSTAT], wr=[tSTAT])
                K.act(S(4), S(3), AF.Sqrt, [tSTAT], [tSTAT])
                K.op(DVE, lambda e: e.reciprocal(out=S(5), in_=S(4)), [tSTAT], [tSTAT])
                K.stt(S(6), S(2), -1.0, S(5), ALU.mult, ALU.mult, [tSTAT], [tSTAT])
                for t in range(NT):
                    K.act(X[:, t, :], X[:, t, :], AF.Identity, [tSTAT] + tX[t], tX[t],
                          bias=STAT[:, 6, t:t + 1], scale=STAT[:, 5, t:t + 1])
                    K.tt(X[:, t, :], X[:, t, :], LNG[:], ALU.mult, [tLNG] + tX[t], tX[t])
                    K.tt(X[:, t, :], X[:, t, :], LNB[:], ALU.add, [tLNB] + tX[t], tX[t])

            def to_fm(X, tX, XT, tXT, scope):
                XB = [scope("XB0", [128, D], BF16), scope("XB1", [128, D], BF16)]; tXB = [Tok(), Tok()]
                for t in range(NT):
                    xb, txb = XB[t % 2], tXB[t % 2]
                    K.act(xb[:], X[:, t, :], AF.Copy, tX[t], [txb])
                    for half in range(2):
                        pb, tpb = bbank()
                        for j in range(8):
                            kc = half * 8 + j
                            K.tr(pb[:, j * 128:(j + 1) * 128], xb[:, kc * 128:(kc + 1) * 128], IDb[:], [txb, tIDb], [tpb],
                                 inc=(j == 7))
                        K.copy(XT[:, half * 8:half * 8 + 8, t * 128:(t + 1) * 128],
                               pb[:, :].rearrange("p (j q) -> p j q", j=8), [tpb], [tXT])

            def scoped():
                s = ExitStack()
                st["sc"] = st.get("sc", 0) + 1
                sid = st["sc"]
                return s, (lambda n, shp, d: s.enter_context(nc.sbuf_tensor(f"{n}_s{sid}", list(shp), d)))

            s23 = es.enter_context(ExitStack())
            sb23 = lambda n, shp, d: s23.enter_context(nc.sbuf_tensor(n, list(shp), d))
            KTO = sb23("KTO", [128, 4, T], BF16); tKTO = Tok()
            IKTO = sb23("IKTO", [128, T], BF16); tIKTO = Tok()
            VMISC = sb23("VMISC", [128, 512], BF16); tVMISC = Tok()
            sxt = es.enter_context(ExitStack())
            XT = sxt.enter_context(nc.sbuf_tensor("XT", [128, 16, T], BF16)); tXT = Tok()
            tX = [[Tok() for _ in range(4)] for _ in range(NT)]

            def fence():
                evs = []
                for e in (PE, ACTE, DVE):
                    if e.cnt > 0:
                        evs.append((e.sem, e.cnt, e.name))
                for q in K.rings.values():
                    for s_, v_ in zip(q["sems"], q["vals"]):
                        if v_ > 0:
                            evs.append((s_, v_, "dma"))
                for e in (PE, ACTE, DVE, POOL, SP):
                    for ev in evs:
                        if ev[2] == e.name and not e.same_raw:
                            continue
                        K.wait_event(e, ev)

            s1, sc1 = scoped()
            with s1:
                X = sc1("X", [128, NT, D], F32)
                K.dma(POOL, XT[:], xTin[:, :, :], wr=[tXT])
                for t in range(NT):
                    K.dma(SP, X[:, t, :], xin[t], wr=tX[t])
                    K.act(X[:, t, :], X[:, t, :], AF.Identity, tX[t], tX[t], scale=ALPHA)
                sf, scf = scoped()
                with sf:
                    ffn(X, tX, XT, tXT, 0, scf)
                fence()
                sl, scl = scoped()
                with sl:
                    ln_tiles(X, tX, 0, scl)
                    to_fm(X, tX, XT, tXT, scl)
                fence()
                for t in range(NT):
                    K.dma(SP, X1s[t], X[:, t, :], rd=tX[t], wr=[tXs])
                K.dma(SP, XTs[:, :, :], XT[:], rd=[tXT], wr=[tXTs])
                tS1 = tX

            fence()

            stop("1")
            s2, sc2 = scoped()
            with s2:
                WF = [sc2(f"WF{i}", [128, 16, 128], BF16) for i in range(6)]; tWF = [Tok() for _ in range(6)]
                wst = dict(k=0)

                def load_w(src):
                    i = wst["k"] % 6
                    wst["k"] += 1
                    K.dma(POOL, WF[i][:], src, wr=[tWF[i]])
                    return WF[i], tWF[i]

                def proj(w, tw, SRC, tSRC, gi):
                    g0, gn = TG[gi]
                    pb, tp = fbank()
                    for kc in range(16):
                        K.mm(pb[:, :gn], w[:, kc, :], SRC[:, kc, g0:g0 + gn], kc == 0, kc == 15, [tw, tSRC], [tp])
                    return pb, tp

                ZT = sc2("ZT", [128, 16, T], BF16); tZT = Tok()
                for cc0 in range(16):
                    K.memset(ZT[:, cc0, :], 0.0, wr=[tZT], eng=DVE)
                CONVW = sc2("CONVW", [128, 16, 3], F32); tCW = Tok()
                SCONV = sc2("SCONV", [128, 16, 16, 2], F32); tSC = Tok()
                K.dma(SP, CONVW[:], convw_d[:, :, :], wr=[tCW])
                K.dma(SP, SCONV[:], sconv_d[:, :, :, :], wr=[tSC])
                GC = sc2("GC", [128, T], F32); tGC = Tok()
                GB = sc2("GB", [128, T], F32); tGB = Tok()
                UE = sc2("UE", [128, 8, 130], F32); US = sc2("US", [128, 16, 6], F32); UM = sc2("UM", [128, 18], F32)
                tU = Tok()
                CVB = sc2("CVB", [128, 8, 128], F32); CVS = sc2("CVS", [128, 16, 4], F32); CVM = sc2("CVM", [128, 16], F32)
                tCV = Tok()
                UTM_M = sc2("UTM_M", [128, D], F32); UTM_7 = sc2("UTM_7", [128, D], F32); tUTM, tUT7 = Tok(), Tok()
                TMPG = sc2("TMPG", [128, 128], F32); tTMPG = Tok()
                K.memset(UM[:], 0.0, wr=[tU])
                for cc in range(16):
                    wgc, twgc = load_w(wfm[FM_GC + cc]); wh, twh = load_w(wfm[FM_H + cc]); wgb, twgb = load_w(wfm[FM_GB + cc])
                    for gi in range(3):
                        g0, gn = TG[gi]
                        pb, tp = proj(wgc, twgc, XT, tXT, gi)
                        K.act(GC[:, g0:g0 + gn], pb[:, :gn], AF.Copy, [tp], [tGC])
                    for gi in range(3):
                        g0, gn = TG[gi]
                        pb, tp = proj(wh, twh, XT, tXT, gi)
                        if gi < 2:
                            K.tt(UE[:, 4 * gi:4 * gi + 4, 2:130], GC[:, g0:g0 + 512].rearrange("p (b q) -> p b q", b=4),
                                 pb[:, :].rearrange("p (b q) -> p b q", b=4), ALU.mult, [tp, tGC], [tU])
                        else:
                            K.tt(UM[:, 2:18], GC[:, 1024:1040], pb[:, 0:16], ALU.mult, [tp, tGC], [tU])
                            K.tt(US[:, :, 2:6], GC[:, 1040:1104].rearrange("p (s q) -> p s q", q=4),
                                 pb[:, 16:80].rearrange("p (s q) -> p s q", q=4), ALU.mult, [tp, tGC], [tU])
                            K.tt(UE[:, :, 0:2], GC[:, 1104:1120].rearrange("p (b q) -> p b q", q=2),
                                 pb[:, 80:96].rearrange("p (b q) -> p b q", q=2), ALU.mult, [tp, tGC], [tU])
                    K.copy(US[:, :, 0:2], SCONV[:, cc, :, :], [tSC], [tU])
                    for gi in range(3):
                        g0, gn = TG[gi]
                        pb, tp = proj(wgb, twgb, XT, tXT, gi)
                        K.act(GB[:, g0:g0 + gn], pb[:, :gn], AF.Copy, [tp], [tGB])
                    w0, w1, w2 = (CONVW[:, cc, j:j + 1] for j in range(3))
                    for (cv, src, a, wdt) in ((CVB[:], lambda o: UE[:, :, o:o + 128], 0, 128),
                                              (CVS[:], lambda o: US[:, :, o:o + 4], 0, 4),
                                              (CVM[:], lambda o: UM[:, o:o + 16], 0, 16)):
                        K.ts(cv, src(2), w2, None, ALU.mult, rd=[tU, tCW], wr=[tCV])
                        K.stt(cv, src(1), w1, cv, ALU.mult, ALU.add, [tU, tCW, tCV], [tCV])
                        K.stt(cv, src(0), w0, cv, ALU.mult, ALU.add, [tU, tCW, tCV], [tCV])
                    K.tt(ZT[:, cc, 0:1024].rearrange("p (b q) -> p b q", b=8), GB[:, 0:1024].rearrange("p (b q) -> p b q", b=8),
                         CVB[:], ALU.mult, [tGB, tCV], [tZT])
                    K.tt(ZT[:, cc, 1024:1040], GB[:, 1024:1040], CVM[:], ALU.mult, [tGB, tCV], [tZT])
                    K.tt(ZT[:, cc, 1040:1104].rearrange("p (s q) -> p s q", q=4), GB[:, 1040:1104].rearrange("p (s q) -> p s q", q=4),
                         CVS[:], ALU.mult, [tGB, tCV], [tZT])
                    for (ti, UT, tUT) in ((8, UTM_M, tUTM), (7, UTM_7, tUT7)):
                        pg2, tpg2 = fbank()
                        for kc in range(16):
                            K.mm(pg2[:, 0:128], XT[:, kc, ti * 128:(ti + 1) * 128], wgc[:, kc, :], kc == 0, kc == 15, [tXT, twgc], [tpg2])
                        ph2, tph2 = fbank()
                        for kc in range(16):
                            K.mm(ph2[:, 0:128], XT[:, kc, ti * 128:(ti + 1) * 128], wh[:, kc, :], kc == 0, kc == 15, [tXT, twh], [tph2])
                        K.act(TMPG[:, :], pg2[:, 0:128], AF.Copy, [tpg2], [tTMPG])
                        K.tt(UT[:, cc * 128:(cc + 1) * 128], TMPG[:, :], ph2[:, 0:128], ALU.mult, [tTMPG, tph2], [tUT])
                for s_i in range(16):
                    K.dma(SP, ust_o[2 * s_i:2 * s_i + 2, :], UTM_M[18 + 4 * s_i:20 + 4 * s_i, :], rd=[tUTM], is_out=True)
                K.dma(SP, ust_o[32:34, :], UTM_7[126:128, :], rd=[tUT7], is_out=True)
                stop("2a")
                SGT = [sc2("SGT0", [128, 512], F32), sc2("SGT1", [128, 512], F32)]; tSGT = [Tok(), Tok()]
                MCC = [sc2("MCC0", [128, T], BF16), sc2("MCC1", [128, T], BF16)]; tMCC = [Tok(), Tok()]
                n = 0
                for oc in range(16):
                    wc_, twc = load_w(wco[oc]); wg_, twg = load_w(wfm[FM_GCONV + oc])
                    mcc, tmcc = MCC[oc % 2], tMCC[oc % 2]
                    for gi in range(3):
                        g0, gn = TG[gi]
                        pg, tpg = proj(wg_, twg, XT, tXT, gi)
                        py, tpy = proj(wc_, twc, ZT, tZT, gi)
                        sg, tsg = SGT[n % 2], tSGT[n % 2]; n += 1
                        K.act(sg[:, :gn], pg[:, :gn], AF.Sigmoid, [tpg], [tsg])
                        K.tt(mcc[:, g0:g0 + gn], sg[:, :gn], py[:, :gn], ALU.mult, [tsg, tpy], [tmcc])
                    K.dma(SP, mcTs[oc], mcc[:], rd=[tmcc], wr=[tmcTs])
                stop("2b")
                for (base, cnt, dst, tdst) in ((FM_Q, 16, qTs, tqTs), (FM_IQ, 8, iqTs, tiqTs)):
                    for h in range(cnt):
                        w_, tw_ = load_w(wfm[base + h])
                        mcc, tmcc = MCC[h % 2], tMCC[h % 2]
                        for gi in range(3):
                            g0, gn = TG[gi]
                            pb, tp = proj(w_, tw_, XT, tXT, gi)
                            K.act(mcc[:, g0:g0 + gn], pb[:, :gn], AF.Copy, [tp], [tmcc])
                        K.dma(SP, dst[h], mcc[:], rd=[tmcc], wr=[tdst])
            fence()
            stop("2c")
            s2b, sc2b = scoped()
            tagki, tagvi, tagii, tagko, tagvo, tagio = (Tok() for _ in range(6))
            with s2b:
                WF2 = [sc2b(f"WG{i}", [128, 16, 128], BF16) for i in range(2)]; tWF2 = [Tok(), Tok()]
                for g in range(5):
                    w_, tw_ = WF2[g % 2], tWF2[g % 2]
                    K.dma(POOL, w_[:], wfm[FM_KT + g] if g < 4 else wfm[FM_IK], wr=[tw_])
                    for gi in range(3):
                        g0, gn = TG[gi]
                        pb, tp = fbank()
                        for kc in range(16):
                            K.mm(pb[:, :gn], w_[:, kc, :], XT[:, kc, g0:g0 + gn], kc == 0, kc == 15, [tw_, tXT], [tp])
                        if g < 4:
                            K.act(KTO[:, g, g0:g0 + gn], pb[:, :gn], AF.Copy, [tp], [tKTO])
                        else:
                            K.act(IKTO[:, g0:g0 + gn], pb[:, :gn], AF.Copy, [tp], [tIKTO])
                stop("2c1")
                for g in range(4):
                    K.dma(SP, agki[g * 128:(g + 1) * 128, :], KTO[:, g, 0:1024], rd=[tKTO], wr=[tagki])
                stop("2c2")
                K.dma(SP, agii[:, :], IKTO[:, 0:1024], rd=[tIKTO], wr=[tagii])
                stop("2d")
                WK = sc2b("WK", [128, 16, 512], BF16); WV = sc2b("WV", [128, 16, 512], BF16); WI = sc2b("WI", [128, 16, 80], BF16)
                tWK, tWV, tWI = Tok(), Tok(), Tok()
                K.dma(POOL, WK[:], wk_d[:, :, :], wr=[tWK]); K.dma(POOL, WV[:], wv_d[:, :, :], wr=[tWV])
                K.dma(POOL, WI[:], wikw_d[:, :, :], wr=[tWI])
                KTM = [sc2b(f"KTM{i}", [128, 512], F32) for i in range(2)]; tKTM = [Tok(), Tok()]
                VTM = [sc2b(f"VTM{i}", [128, 512], F32) for i in range(2)]; tVTM = [Tok(), Tok()]
                VBF = [sc2b(f"VBF{i}", [128, 512], BF16) for i in range(2)]; tVBF = [Tok(), Tok()]
                ITM = [sc2b(f"ITM{i}", [128, 80], F32) for i in range(2)]; tITM = [Tok(), Tok()]
                for t in range(NT):
                    xs = lambda kc: XT[:, kc, t * 128:(t + 1) * 128]
                    i2 = t % 2
                    pk, tpk = fbank()
                    for kc in range(16):
                        K.mm(pk[:, :], xs(kc), WK[:, kc, :], kc == 0, kc == 15, [tXT, tWK], [tpk])
                    K.act(KTM[i2][:], pk[:, :], AF.Copy, [tpk], [tKTM[i2]])
                    K.dma(SP, k_blk[t] if t < 8 else k_misc[:, :], KTM[i2][:], rd=[tKTM[i2]], is_out=True)
                    stop(f"t{t}k")
                    pv, tpv = fbank()
                    for kc in range(16):
                        K.mm(pv[:, :], xs(kc), WV[:, kc, :], kc == 0, kc == 15, [tXT, tWV], [tpv])
                    K.act(VTM[i2][:], pv[:, :], AF.Copy, [tpv], [tVTM[i2]])
                    K.dma(SP, v_blk[t] if t < 8 else v_misc[:, :], VTM[i2][:], rd=[tVTM[i2]], is_out=True)
                    if t < 8:
                        K.copy(VBF[i2][:], VTM[i2][:], [tVTM[i2]], [tVBF[i2]])
                        K.dma(SP, agvi[t * 128:(t + 1) * 128, :], VBF[i2][:], rd=[tVBF[i2]], wr=[tagvi])
                    else:
                        K.copy(VMISC[:], VTM[i2][:], [tVTM[i2]], [tVMISC])
                        K.dma(SP, vns[:, :], VMISC[:, :], rd=[tVMISC], wr=[tagvi])
                    stop(f"t{t}v")
                    pi, tpi = fbank()
                    for kc in range(16):
                        K.mm(pi[:, 0:80], xs(kc), WI[:, kc, :], kc == 0, kc == 15, [tXT, tWI], [tpi])
                    K.act(ITM[i2][:], pi[:, 0:80], AF.Copy, [tpi], [tITM[i2]])
                    K.dma(SP, ik_blk[t] if t < 8 else ik_misc[:, :], ITM[i2][:, 0:64], rd=[tITM[i2]], is_out=True)
                    stop(f"t{t}i")
                    K.ts(WQ[:, t, :], ITM[i2][:, 64:80], 1.0 / 32.0, None, ALU.mult, rd=[tITM[i2]], wr=[tWQ])
                    stop(f"t{t}w")
                stop("2e")
                grp = [[0, 1, 2, 3], [4, 5, 6, 7]]
                v128 = lambda h, p: h.ap().rearrange("(p a) c -> p (a c)", p=p).opt()
                if os.environ.get("KNOCOLL", "0") != "1":
                    K.coll("AllGather", [v128(agki, 128)], [v128(agko, 512)], grp, [tagki], [tagko])
                    K.coll("AllGather", [v128(agvi, 128)], [v128(agvo, 512)], grp, [tagvi], [tagvo])
                    K.coll("AllGather", [v128(agii, 128)], [v128(agio, 512)], grp, [tagii], [tagio])
            fence()
            stop("2")
            sxt.close()

            NENT2 = 57
            s3, sc3 = scoped()
            with s3:
                KT = sc3("KT", [128, 4, 4112], BF16); tKT = Tok()
                VA = sc3("VA", [128, 33, 512], BF16); tVA = Tok()
                IKT = sc3("IKT", [128, 4112], BF16); tIKT = Tok()
                I = sc3("I", [128, 4112], F32)
                MNEGT = sc3("MNEGT", [128, 33, 128], BF16); tMT = Tok()
                JUNK = sc3("JUNK", [128, 4112], BF16); tJ = Tok()
                MNEG, tMNEG = JUNK, tJ
                KPOS = sc3("KPOS", [128, 4112], BF16); tKPOS = Tok()
                QT = sc3("QT", [128, 16, 128], BF16); tQT = Tok()
                IQT = sc3("IQT", [128, 8, 128], BF16); tIQT = Tok()
                OTS = sc3("OTS", [128, 16, 128], BF16); tOTS = Tok()
                PT = [sc3(f"PT{i}", [128, 512], BF16) for i in range(2)]; tPT = [Tok(), Tok()]
                R = [sc3(f"R{i}", [128, 512], F32) for i in range(2)]; tR = [Tok(), Tok()]
                DIAGX = sc3("DIAGX", [128, 16, 128], BF16); tDX = Tok()
                IDT16 = sc3("IDT16", [128, 16, 128], BF16); tI16 = Tok()
                ALIBL = sc3("ALIBL", [5, NENT2, 128], BF16); tAL = Tok()
                ALIBR5 = sc3("ALIBR5", [5, 16, 128], BF16); tAR = Tok()
                CM = sc3("CM", [128, 512], F32); CMS = sc3("CMS", [4, 4], F32); tCM = Tok()
                POW2 = sc3("POW2", [128, NITER + 1], F32); MS = sc3("MS", [128, 16], F32); T0T = sc3("T0T", [128, 9], F32)
                tCST = Tok()
                SM = sc3("SM", [128, 8], F32); CNT = sc3("CNT", [128, NITER], F32); WTAB = sc3("WTAB", [128, NITER + 1], F32)
                RTM = sc3("RTM", [128, 16], F32); tSM = Tok()
                tRINV = Tok()
                tWQS = Tok()
                wqs_d = dscr("wqs_d", [64, 16], F32); twqsd = Tok()
                K.dma(POOL, KPOS[:], kpos_d[0:1, :].partition_broadcast(128), wr=[tKPOS])
                K.dma(POOL, IDT16[:], idt16_d[:, :, :], wr=[tI16])
                K.dma(POOL, ALIBL[:], alibl_d[:, :, :], wr=[tAL])
                K.dma(POOL, ALIBR5[1:5, :, :], alibr_d[:, :].rearrange("r (h q) -> r h q", h=16), wr=[tAR])
                K.dma(SP, CM[:], cmask_d[:, :], wr=[tCM]); K.dma(SP, CMS[:], cms_d[:, :], wr=[tCM])
                K.dma(SP, POW2[:], pow2_d[0:1, :].partition_broadcast(128), wr=[tCST])
                K.dma(SP, MS[:], ms_d[0:1, :].partition_broadcast(128), wr=[tCST])
                K.dma(SP, T0T[:], t0_d[0:1, :].partition_broadcast(128), wr=[tCST])
                K.dma(SP, wqs_d[:, :], WQ[16:80, 8, :], rd=[tWQ], wr=[twqsd])
                for g in range(4):
                    for r in range(4):
                        K.dma(SP, KT[:, g, 16:4112].rearrange("d (i r p) -> d i r p", r=4, p=128)[:, :, r, :],
                              agko[r * 512 + g * 128:r * 512 + (g + 1) * 128, :].rearrange("d (i p) -> d i p", p=128),
                              rd=[tagko], wr=[tKT])
                for r in range(4):
                    K.dma(SP, VA[:, 1:33, :].rearrange("p (i r) f -> p i r f", r=4)[:, :, r, :],
                          agvo[r * 1024:(r + 1) * 1024, :].rearrange("(i p) f -> p i f", p=128), rd=[tagvo], wr=[tVA])
                    K.dma(SP, IKT[:, 16:4112].rearrange("d (i r p) -> d i r p", r=4, p=128)[:, :, r, :],
                          agio[r * 128:(r + 1) * 128, :].rearrange("d (i p) -> d i p", p=128), rd=[tagio], wr=[tIKT])
                K.copy(KT[:, :, 0:16], KTO[:, :, 1024:1040], [tKTO], [tKT])
                K.copy(IKT[:, 0:16], IKTO[:, 1024:1040], [tIKTO], [tIKT])
                K.copy(VA[0:16, 0, :], VMISC[0:16, :], [tVMISC], [tVA])
                ALR = ALIBR5
                ACCS = sc3("ACCS", [128, 4, 512], BF16); RSS = sc3("RSS", [128, 4, 512], F32); tACCS = Tok()
                RINV = RSS[:, 0, :]
                QTb = sc3("QTb", [128, 16, 128], BF16); IQTb = sc3("IQTb", [128, 8, 128], BF16)
                MNEGTb = sc3("MNEGTb", [128, 33, 128], BF16); ALRb = sc3("ALRb", [5, 16, 128], BF16)
                K.dma(POOL, ALRb[1:5, :, :], alibr_d[:, :].rearrange("r (h q) -> r h q", h=16), wr=[tAR])
                CTX = [dict(QT=QT, tQT=tQT, IQT=IQT, tIQT=tIQT, MNEGT=MNEGT, tMT=tMT, ALR=ALIBR5, tAR=tAR),
                       dict(QT=QTb, tQT=Tok(), IQT=IQTb, tIQT=Tok(), MNEGT=MNEGTb, tMT=Tok(), ALR=ALRb, tAR=tAR)]

                v3g = lambda ap, rows, nq: ap[0:rows, :].rearrange("p (h q) -> p h q", h=4)[:, :, 0:nq]
                cnt = dict(n=0, m=0)

                def indexer(nq, q0, L, wq_of_h, tIr, first=True, cx=None):
                    cx = cx or CTX[0]
                    IQT, tIQT = cx['IQT'], cx['tIQT']
                    ranges = [(k0, min(512, L - k0)) for k0 in range(0, L, 512)]
                    for h in range(16):
                        for ri, (k0, kn) in enumerate(ranges):
                            e2, p = h % 2, h // 2
                            pb, tp = fbank(0, 4)
                            K.mm(pb[0:nq, 0:kn], IQT[64 * e2:64 * e2 + 64, p, q0:q0 + nq], IKT[64 * e2:64 * e2 + 64, k0:k0 + kn],
                                 True, True, [tIQT, tIKT], [tp])
                            r_, tr_ = R[cnt["n"] % 2], tR[cnt["n"] % 2]; cnt["n"] += 1
                            K.act(r_[0:nq, 0:kn], pb[0:nq, 0:kn], AF.Relu, [tp], [tr_])
                            if h == 0 and first:
                                K.ts(I[0:nq, k0:k0 + kn], r_[0:nq, 0:kn], wq_of_h(0), None, ALU.mult, rd=[tr_, tWQ, tWQS], wr=[tIr[ri]])
                            else:
                                K.stt(I[0:nq, k0:k0 + kn], r_[0:nq, 0:kn], wq_of_h(h), I[0:nq, k0:k0 + kn], ALU.mult, ALU.add,
                                      [tr_, tWQ, tWQS, tIr[ri]], [tIr[ri]])

                def select(nq, chunks, L, cm_ap, cm_w, t0col, tIr, cx=None, do_b=True):
                    cx = cx or CTX[0]
                    MNEGT, tMT, ALR, tAR = cx['MNEGT'], cx['tMT'], cx['ALR'], cx['tAR']
                    sm = lambda c: SM[0:nq, c:c + 1]
                    Iv = I[0:nq, 0:L]
                    K.red(sm(0), Iv, ALU.min, tIr, [tSM]); K.red(sm(1), Iv, ALU.max, tIr, [tSM])
                    K.tt(I[0:nq, L - cm_w:L], I[0:nq, L - cm_w:L], cm_ap, ALU.add, [tCM] + tIr, tIr)
                    K.tt(sm(2), sm(1), sm(0), ALU.subtract, [tSM], [tSM])
                    K.stt(sm(0), sm(2), -1.0 / 1024.0, sm(0), ALU.mult, ALU.add, [tSM], [tSM])
                    K.ts(WTAB[0:nq, :], POW2[0:nq, :], sm(2), None, ALU.mult, rd=[tSM, tCST], wr=[tSM])
                    K.memset(CNT[0:nq, :], 0.0, wr=[tSM])
                    K.tt(sm(4), sm(0), WTAB[0:nq, 0:1], ALU.add, [tSM], [tSM])
                    for k in range(NITER):
                        K.ts(JUNK[0:nq, 0:L], Iv, sm(4), 0.0, ALU.is_ge, ALU.add, rd=[tSM] + tIr, wr=[tJ, tSM],
                             accum_out=CNT[0:nq, k:k + 1])
                        K.stt(sm(5), CNT[0:nq, k:k + 1], 255.5, WTAB[0:nq, k:k + 1], ALU.is_ge, ALU.mult, [tSM], [tSM])
                        K.stt(sm(4), sm(4), WTAB[0:nq, k + 1:k + 2], sm(5), ALU.subtract, ALU.add, [tSM], [tSM])
                    K.tt(sm(3), sm(4), WTAB[0:nq, NITER:NITER + 1], ALU.subtract, [tSM], [tSM])
                    K.stt(JUNK[0:nq, 0:L], Iv, sm(3), KPOS[0:nq, 0:L], ALU.is_ge, ALU.mult, [tSM, tKPOS] + tIr, [tJ])
                    K.red(sm(6), JUNK[0:nq, 0:L], ALU.max, [tJ], [tSM])
                    K.ts(MNEG[0:nq, 0:L], Iv, sm(3), -65536.0, ALU.is_lt, ALU.mult, rd=[tSM] + tIr, wr=[tMNEG])
                    K.ts(sm(7), sm(6), -1.0, T0T[0:nq, t0col:t0col + 1], ALU.mult, ALU.add, rd=[tSM, tCST], wr=[tSM])
                    K.ts(RTM[0:nq, :], MS[0:nq, :], sm(7), None, ALU.mult, rd=[tSM, tCST], wr=[tSM])
                    K.tt(DIAGX[0:nq, :, 0:nq], IDT16[0:nq, :, 0:nq],
                         RTM[0:nq, :].rearrange("p (h o) -> p h o", o=1).to_broadcast([nq, 16, nq]), ALU.mult, [tSM, tI16], [tDX])
                    if do_b:
                        select_b(nq, chunks, cx)

                def select_b(nq, chunks, cx):
                    MNEGT, tMT, ALR, tAR = cx['MNEGT'], cx['tMT'], cx['ALR'], cx['tAR']
                    for g in range(4):
                        pb, tp = fbank(0, 4)
                        K.mm(v3g(pb, 1, nq), ONESb[0:nq, 0:1], DIAGX[0:nq, 4 * g:4 * g + 4, 0:nq], True, True, [tONES, tDX], [tp])
                        K.copy(ALR[0:1, 4 * g:4 * g + 4, 0:nq], v3g(pb, 1, nq), [tp], [tAR])
                    for (ci, n_, k0) in chunks:
                        pbb, tpbb = bbank()
                        K.tr(pbb[0:n_, 0:nq], MNEG[0:nq, k0:k0 + n_], IDb[0:nq, 0:nq], [tMNEG, tIDb], [tpbb])
                        K.copy(MNEGT[0:n_, ci, 0:nq], pbb[0:n_, 0:nq], [tpbb], [tMT])

                def attn(nq, q0, chunks, ents, ot_dst, t_ot, cx=None, defer=False):
                    cx = cx or CTX[0]
                    QT, tQT, MNEGT, tMT, ALR, tAR = cx['QT'], cx['tQT'], cx['MNEGT'], cx['tMT'], cx['ALR'], cx['tAR']
                    acc, tacc, rs, trs = FB[4], tFB[4], FB[5], tFB[5]
                    v3 = lambda ap, rows: v3g(ap, rows, nq)
                    for g in range(4):
                        def score(idx):
                            ci, n_, k0 = chunks[idx]
                            sc, tsc = fbank(0, 4)
                            K.mm(v3(sc, n_), KT[:, g, k0:k0 + n_], QT[:, 4 * g:4 * g + 4, q0:q0 + nq], True, False, [tKT, tQT], [tsc])
                            K.mm(v3(sc, n_), ALIBL[0:5, ents[idx], 0:n_], ALR[0:5, 4 * g:4 * g + 4, 0:nq], False, False, [tAL, tAR], [tsc])
                            K.mm(v3(sc, n_), IDb[0:n_, 0:n_],
                                 MNEGT[0:n_, ci, 0:nq].rearrange("p (o q) -> p o q", o=1).to_broadcast([n_, 4, nq]),
                                 False, True, [tIDb, tMT], [tsc])
                            pt, tpt = PT[cnt["m"] % 2], tPT[cnt["m"] % 2]; cnt["m"] += 1
                            K.act(v3(pt, n_), v3(sc, n_), AF.Exp, [tsc], [tpt], bias=C0, scale=SCALE)
                            return pt, tpt
                        cur = score(0)
                        for idx, (ci, n_, k0) in enumerate(chunks):
                            last = idx == len(chunks) - 1
                            nxt = None if last else score(idx + 1)
                            pt, tpt = cur
                            K.mm(v3(acc, 128), VA[0:n_, ci, g * 128:(g + 1) * 128], v3(pt, n_), idx == 0, last, [tVA, tpt], [tacc])
                            K.mm(v3(rs, 128), ONESb[0:n_, :], v3(pt, n_), idx == 0, last, [tONES, tpt], [trs])
                            cur = nxt
                        if defer:
                            K.act(ACCS[:, g, :], acc[:, :], AF.Copy, [tacc], [tACCS])
                            K.act(RSS[:, g, :], rs[:, :], AF.Copy, [trs], [tACCS])
                        else:
                            K.op(DVE, lambda e, o=v3(RINV, 128), i_=v3(rs, 128): e.reciprocal(out=o, in_=i_), [trs], [tRINV])
                            K.tt(ot_dst[:, 4 * g:4 * g + 4, :], v3(acc, 128), v3(RINV, 128), ALU.mult, [tacc, tRINV], [t_ot])

                def finish_attn(ot_dst, t_ot):
                    K.op(DVE, lambda e: e.reciprocal(out=RSS[:, :, :], in_=RSS[:, :, :]), [tACCS], [tACCS])
                    K.tt(ot_dst.rearrange("d (g h) q -> d g (h q)", g=4), ACCS[:, :, :], RSS[:, :, :], ALU.mult, [tACCS], [t_ot])

                slot = {}

                def prep(i):
                    cx = CTX[i % 2]
                    K.dma(SP, cx['QT'][:], qTs[:, :, i * 128:(i + 1) * 128].rearrange("h d t -> d h t"), rd=[tqTs], wr=[cx['tQT']])
                    K.dma(SP, cx['IQT'][:], iqTs[:, :, i * 128:(i + 1) * 128].rearrange("h d t -> d h t"), rd=[tiqTs], wr=[cx['tIQT']])
                    nb = 4 * i + 4
                    chunks = [(0, 16, 0)] + [(1 + j, 128, 16 + 128 * j) for j in range(nb)]
                    ents = [i] + [8 + (j - 4 * i + 28) for j in range(nb)]
                    L = 16 + 128 * nb
                    tIr = [Tok() for _ in range(0, L, 512)]
                    indexer(128, 0, L, lambda h, i=i: WQ[:, i, h:h + 1], tIr, cx=cx)
                    select(128, chunks, L, CM[:, :], 512, i, tIr, cx=cx, do_b=False)
                    slot[i] = (chunks, ents)

                prep(0)
                select_b(128, slot[0][0], CTX[0])
                for i in range(8):
                    if i + 1 < 8:
                        prep(i + 1)
                    chunks, ents = slot[i]
                    attn(128, 0, chunks, ents, OTS[:, :, :], tOTS, cx=CTX[i % 2], defer=True)
                    if i + 1 < 8:
                        select_b(128, slot[i + 1][0], CTX[(i + 1) % 2])
                    finish_attn(OTS[:, :, :], tOTS)
                    K.dma(SP, oTs[:, :, i * 128:(i + 1) * 128].rearrange("h d t -> d h t"), OTS[:], rd=[tOTS], wr=[toTs])
                fence()
                stop("3p")

                K.dma(SP, QT[:], qTs[:, :, 1024:1152].rearrange("h d t -> d h t"), rd=[tqTs], wr=[tQT])
                K.dma(SP, IQT[:], iqTs[:, :, 1024:1152].rearrange("h d t -> d h t"), rd=[tiqTs], wr=[tIQT])
                K.memset(OTS[:], 0.0, wr=[tOTS])
                PTB = sc3("PTB", [128, 256], I32); IOTA = sc3("IOTA", [128, 1], I32); PIDX = sc3("PIDX", [128, 256], I32)
                tPIDX = Tok()
                K.dma(SP, PTB[:], ptab_d[0:1, :].partition_broadcast(128), wr=[tPIDX])
                K.dma(SP, IOTA[:], iota_d[:, :], wr=[tPIDX])
                K.ts(PIDX[:], PTB[:], 128, IOTA[:, 0:1], ALU.mult, ALU.add, rd=[tPIDX], wr=[tPIDX])
                WQ64 = sc3("WQ64", [64, 16], F32); SEL = sc3("SEL", [64, 16], F32); WQM = sc3("WQM", [64, 16, 16], F32)
                CMS64 = sc3("CMS64", [64, 4], F32)
                K.dma(SP, WQ64[:], wqs_d[:, :], rd=[twqsd], wr=[tWQS])
                K.dma(SP, SEL[:], sel_d[:, :], wr=[tWQS])
                K.dma(SP, CMS64[:], cms64_d[:, :], wr=[tCM])
                for s_i in range(16):
                    K.ts(WQM[:, s_i, :], WQ64[:, :], SEL[:, s_i:s_i + 1], None, ALU.mult, rd=[tWQS], wr=[tWQS])
                stop("3s0")
                IKGs = [ACCS[:, :, :].rearrange("p g (a b) -> p (g a) b", b=128), KT[:, 3, 2064:4112].rearrange("p (a b) -> p a b", b=128)]
                tIKG = [Tok(), Tok()]
                KGs = [VA[:, 17:25, :], VA[:, 25:33, :]]; tKG = [Tok(), Tok()]
                schunks = [(p, 128, 128 * p) for p in range(16)] + [(16, 4, 2048)]
                sents = [40 + p for p in range(16)] + [56]
                tKTc = [Tok() for _ in range(17)]; tVAc = [Tok() for _ in range(17)]

                def gather(dst_ap, tdst, src, col):
                    K.dma(POOL, None, None, rd=[tPIDX], wr=[tdst],
                          fn=lambda e, o=dst_ap, s_=src, c_=col: e.indirect_dma_start(
                              out=o, out_offset=None, in_=s_, in_offset=bass.IndirectOffsetOnAxis(ap=PIDX[:, c_:c_ + 1], axis=0)))

                LS = 2052
                tIs = [Tok() for _ in range(0, LS, 512)]
                for s_i in range(16):
                    ikg = IKGs[s_i % 2]
                    tik = tIKG[s_i % 2]
                    for p in range(16):
                        gather(ikg[:, p, 0:64], tik, cik_d[:, :], s_i * 16 + p)
                    K.copy(ikg[:, :, 64:128], ikg[:, :, 0:64], [tik], [tik])
                    for half in range(2):
                        pbb, tpbb = bbank()
                        for j in range(8):
                            K.tr(pbb[:, j * 128:(j + 1) * 128], ikg[:, half * 8 + j, :], IDb[:], [tik, tIDb], [tpbb], inc=(j == 7))
                        K.copy(IKT[:, half * 1024:(half + 1) * 1024], pbb[:, :], [tpbb], [tIKT])
                    c0 = 1040 + 4 * s_i
                    K.copy(IKT[:, 2048:2052], IKTO[:, c0:c0 + 4], [tIKTO], [tIKT])
                    indexer(64, 16, LS, lambda h, s_i=s_i: WQM[:, s_i, h:h + 1], tIs, first=(s_i == 0))
                stop("3s1")
                select(64, schunks, LS, CMS64[:, :], 4, 8, tIs)
                stop("3s2")
                acc, tacc, rs, trs = FB[4], tFB[4], FB[5], tFB[5]
                v16 = lambda ap, rows: ap[0:rows, 0:64].rearrange("p (h q) -> p h q", h=16)
                for s_i in range(int(os.environ.get("KNS3", "16"))):
                    qs = 16 + 4 * s_i
                    for half in range(2):
                        kg, tkg = KGs[half], tKG[half]
                        for p8 in range(8):
                            gather(kg[:, p8, :], tkg, ck_d[:, :], s_i * 16 + half * 8 + p8)
                    for p in range(16):
                        gather(VA[:, p, :], tVAc[p], cv_d[:, :], s_i * 16 + p)
                    K.dma(SP, VA[0:4, 16, :], vns[16 + 4 * s_i:20 + 4 * s_i, :], rd=[tagvi], wr=[tVAc[16]])
                    for p in range(16):
                        kg, tkg = KGs[p // 8], tKG[p // 8]
                        pbb, tpbb = bbank()
                        for g in range(4):
                            K.tr(pbb[:, g * 128:(g + 1) * 128], kg[:, p % 8, g * 128:(g + 1) * 128], IDb[:], [tkg, tIDb], [tpbb], inc=(g == 3))
                        K.copy(KT[:, :, 128 * p:128 * p + 128], pbb[:, 0:512].rearrange("d (g k) -> d g k", g=4), [tpbb], [tKTc[p]])
                    K.copy(KT[:, :, 2048:2052], KTO[:, :, qs + 1024:qs + 1028], [tKTO], [tKTc[16]])
                    def sscore(idx):
                        ci, n_, k0 = schunks[idx]
                        sc, tsc = fbank(0, 4)
                        K.mm(v16(sc, n_), ALIBL[0:5, sents[idx], 0:n_], ALR[0:5, 0:16, 4 * s_i:4 * s_i + 4], True, False, [tAL, tAR], [tsc])
                        for g in range(4):
                            K.mm(v16(sc, n_)[:, 4 * g:4 * g + 4, :], KT[:, g, k0:k0 + n_], QT[:, 4 * g:4 * g + 4, qs:qs + 4], False, False,
                                 [tKTc[ci], tQT], [tsc])
                        K.mm(v16(sc, n_), IDb[0:n_, 0:n_],
                             MNEGT[0:n_, ci, 4 * s_i:4 * s_i + 4].rearrange("p (o q) -> p o q", o=1).to_broadcast([n_, 16, 4]),
                             False, True, [tIDb, tMT], [tsc])
                        pt, tpt = PT[cnt["m"] % 2], tPT[cnt["m"] % 2]; cnt["m"] += 1
                        K.act(v16(pt, n_), v16(sc, n_), AF.Exp, [tsc], [tpt], bias=C0, scale=SCALE)
                        return pt, tpt
                    cur = sscore(0)
                    for idx, (ci, n_, k0) in enumerate(schunks):
                        last = idx == len(schunks) - 1
                        nxt = None if last else sscore(idx + 1)
                        pt, tpt = cur
                        cur = nxt
                        for g in range(4):
                            K.mm(v16(acc, 128)[:, 4 * g:4 * g + 4, :], VA[0:n_, ci, g * 128:(g + 1) * 128], v16(pt, n_)[:, 4 * g:4 * g + 4, :],
                                 idx == 0, last, [tVAc[ci], tpt], [tacc])
                        K.mm(v16(rs, 128), ONESb[0:n_, :], v16(pt, n_), idx == 0, last, [tONES, tpt], [trs])
                    K.op(DVE, lambda e, o=v16(RINV, 128), i_=v16(rs, 128): e.reciprocal(out=o, in_=i_), [trs], [tRINV])
                    K.tt(OTS[:, :, qs:qs + 4], v16(acc, 128), v16(RINV, 128), ALU.mult, [tacc, tRINV], [tOTS])
                K.dma(SP, oTs[:, :, 1024:1152].rearrange("h d t -> d h t"), OTS[:], rd=[tOTS], wr=[toTs])
            fence()
            stop("3")
            s23.close()
            mgTs = dscr("mgTs", [16, 128, T], BF16); tmgTs = Tok()
            s4a, sc4a = scoped()
            with s4a:
                XT = sc4a("XT4", [128, 16, T], BF16); tXT = Tok()
                OT = sc4a("OT", [128, 16, T], BF16); tOT = Tok()
                K.dma(SP, OT[:], oTs[:, :, :].rearrange("h d t -> d h t"), rd=[toTs], wr=[tOT])
                K.dma(SP, XT[:], XTs[:, :, :], rd=[tXTs], wr=[tXT])
                WA = [sc4a(f"WA{i}", [128, 16, 128], BF16) for i in range(4)]; tWA = [Tok() for _ in range(4)]
                SGT = [sc4a("SGU0", [128, 512], F32), sc4a("SGU1", [128, 512], F32)]; tSGT = [Tok(), Tok()]
                MA = [sc4a("MA0", [128, 512], F32), sc4a("MA1", [128, 512], F32)]; tMA = [Tok(), Tok()]
                MCC = [sc4a("MCD0", [128, T], BF16), sc4a("MCD1", [128, T], BF16)]; tMCC = [Tok(), Tok()]
                MGC = [sc4a("MGC0", [128, T], BF16), sc4a("MGC1", [128, T], BF16)]; tMGC = [Tok(), Tok()]
                n = 0
                for oc in range(16):
                    wa, twa = WA[(2 * oc) % 4], tWA[(2 * oc) % 4]
                    wg_, twg = WA[(2 * oc + 1) % 4], tWA[(2 * oc + 1) % 4]
                    K.dma(POOL, wa[:], wao[oc], wr=[twa]); K.dma(POOL, wg_[:], wfm[FM_GATTN + oc], wr=[twg])
                    mcc, tmcc = MCC[oc % 2], tMCC[oc % 2]
                    mgc, tmgc = MGC[oc % 2], tMGC[oc % 2]
                    K.dma(SP, mcc[:], mcTs[oc], rd=[tmcTs], wr=[tmcc])
                    for gi in range(3):
                        g0, gn = TG[gi]
                        pg, tpg = fbank()
                        for kc in range(16):
                            K.mm(pg[:, :gn], wg_[:, kc, :], XT[:, kc, g0:g0 + gn], kc == 0, kc == 15, [twg, tXT], [tpg])
                        py, tpy = fbank()
                        for kc in range(16):
                            K.mm(py[:, :gn], wa[:, kc, :], OT[:, kc, g0:g0 + gn], kc == 0, kc == 15, [twa, tOT], [tpy])
                        sg, tsg = SGT[n % 2], tSGT[n % 2]; ma, tma = MA[n % 2], tMA[n % 2]; n += 1
                        K.act(sg[:, :gn], pg[:, :gn], AF.Sigmoid, [tpg], [tsg])
                        K.tt(ma[:, :gn], sg[:, :gn], py[:, :gn], ALU.mult, [tsg, tpy], [tma])
                        K.tt(mgc[:, g0:g0 + gn], ma[:, :gn], mcc[:, g0:g0 + gn], ALU.add, [tma, tmcc], [tmgc])
                    K.dma(SP, mgTs[oc], mgc[:], rd=[tmgc], wr=[tmgTs])
            fence()
            s5, sc5 = scoped()
            with s5:
                XT = sc5("XT5", [128, 16, T], BF16); tXT = Tok()
                X = sc5("X2", [128, NT, D], F32)
                tX = [[Tok() for _ in range(4)] for _ in range(NT)]
                for t in range(NT):
                    K.dma(SP, X[:, t, :], X1s[t], rd=[tXs], wr=tX[t])
                    K.act(X[:, t, :], X[:, t, :], AF.Identity, tX[t], tX[t], scale=ALPHA)
                s5a, sc5a = scoped()
                with s5a:
                    MERGED = sc5a("MERGED", [128, 16, T], BF16); tMG = Tok()
                    K.dma(SP, MERGED[:], mgTs[:, :, :].rearrange("c p t -> p c t"), rd=[tmgTs], wr=[tMG])
                    WO = [sc5a("WO0", [128, 16, 256], BF16), sc5a("WO1", [128, 16, 256], BF16)]; tWO = [Tok(), Tok()]
                    for d8 in range(8):
                        wo_, two = WO[d8 % 2], tWO[d8 % 2]
                        K.dma(POOL, wo_[:], wo_d[d8], wr=[two])
                        for t in range(NT):
                            po, tpo = fbank()
                            for oc in range(16):
                                K.mm(po[:, 0:256], MERGED[:, oc, t * 128:(t + 1) * 128], wo_[:, oc, :], oc == 0, oc == 15, [tMG, two], [tpo])
                            xs = X[:, t, d8 * 256:(d8 + 1) * 256]
                            K.tt(xs, po[:, 0:256], xs, ALU.add, [tpo, tX[t][d8 // 2]], [tX[t][d8 // 2]])
                fence()
                sl, scl = scoped()
                with sl:
                    ln_tiles(X, tX, 1, scl)
                    to_fm(X, tX, XT, tXT, scl)
                fence()
                for t in range(NT):
                    K.act(X[:, t, :], X[:, t, :], AF.Identity, tX[t], tX[t], scale=ALPHA)
                sf, scf = scoped()
                with sf:
                    ffn(X, tX, XT, tXT, 1, scf)
                fence()
                sl, scl = scoped()
                with sl:
                    ln_tiles(X, tX, 2, scl)
                for t in range(NT):
                    K.dma(SP, y_blk[t] if t < 8 else y_misc[:, :], X[:, t, :], rd=tX[t], is_out=True)
            K.finish()
    except _Stop:
        pass
    return nc


_NC_CACHE = {}


def _bf16_round(x):
    x = np.asarray(x, dtype=np.float32)
    u = x.view(np.uint32).astype(np.uint64)
    u = ((u + 0x7FFF + ((u >> 16) & 1)) >> 16) << 16
    return u.astype(np.uint32).view(np.float32)


def kernel(x_prompt, x_sample, cache_k, cache_v, cache_idx_k, state_conv, page_table, meta_tokens,
           w_in, conv_w, w_conv_out, w_attn_out, w_o, ffn1_w_gu, ffn1_w_down, ffn2_w_gu, ffn2_w_down,
           ln1_g, ln1_b, ln2_g, ln2_b, ln3_g, ln3_b):
    f32 = np.float32
    A = lambda a: np.asarray(a)
    x_prompt, x_sample, meta_tokens = A(x_prompt), A(x_sample), A(meta_tokens)
    w_in0 = A(w_in)[0]
    def gu(w):
        return np.ascontiguousarray(A(w)[0].reshape(16, 128, 2, NU, 256).transpose(3, 1, 0, 2, 4))

    def dn(w):
        return np.ascontiguousarray(A(w)[0].reshape(NU, 2, 128, D).transpose(0, 2, 1, 3))

    def fmchunks(w):
        n = w.shape[1] // 128
        return np.ascontiguousarray(w.reshape(16, 128, n, 128).transpose(2, 1, 0, 3))

    cols = []
    for base, cnt in ((0, 16), (2048, 16), (4096, 16), (6144, 16), (8192, 4), (9216, 8)):
        for j in range(cnt):
            cols.append(np.arange(base + 128 * j, base + 128 * j + 128))
    cols.append(np.concatenate([np.arange(10240, 10304), np.arange(10240, 10304)]))
    for base in (10320, 12368):
        for j in range(16):
            cols.append(np.arange(base + 128 * j, base + 128 * j + 128))
    cols = np.concatenate(cols)
    wfm = fmchunks(w_in0[:, cols])
    tm = lambda w: np.ascontiguousarray(w.reshape(16, 128, w.shape[1]).transpose(1, 0, 2))
    shared = {
        "wgu1": gu(ffn1_w_gu), "wgu2": gu(ffn2_w_gu), "wd1": dn(ffn1_w_down), "wd2": dn(ffn2_w_down),
        "wfm": wfm, "wk": tm(w_in0[:, 8192:8704]), "wv": tm(w_in0[:, 8704:9216]), "wikw": tm(w_in0[:, 10240:10320]),
        "wco": fmchunks(A(w_conv_out)[0]), "wao": fmchunks(A(w_attn_out)[0]),
        "wo": np.ascontiguousarray(A(w_o)[0].reshape(16, 128, 8, 256).transpose(2, 1, 0, 3)),
        "lnp": np.stack([A(ln1_g)[0], A(ln1_b)[0], A(ln2_g)[0], A(ln2_b)[0], A(ln3_g)[0], A(ln3_b)[0]]).astype(f32),
        "convw": np.ascontiguousarray(A(conv_w)[0].reshape(3, 16, 128).transpose(2, 1, 0)),
        "ck": A(cache_k)[0][:NPOOL].reshape(NPOOL * 128, 512), "cv": A(cache_v)[0][:NPOOL].reshape(NPOOL * 128, 512),
        "cik": A(cache_idx_k)[0][:NPOOL].reshape(NPOOL * 128, 64),
        "iota": np.arange(128, dtype=np.int32).reshape(128, 1),
        "ident": np.eye(128, dtype=f32),
        "cms": np.where(np.arange(4)[None, :] <= np.arange(4)[:, None], 0.0, -1e30).astype(f32),
        "sel": np.repeat(np.eye(16, dtype=f32), 4, axis=0),
        "cms64": np.tile(np.where(np.arange(4)[None, :] <= np.arange(4)[:, None], 0.0, -1e30).astype(f32), (16, 1)),
        "pow2": (2.0 ** -(np.arange(NITER + 1) + 1.0)).astype(f32).reshape(1, NITER + 1),
        "kpos": np.arange(4112, dtype=f32).reshape(1, 4112),
        "idt16": np.ascontiguousarray(np.broadcast_to(np.eye(128, dtype=f32)[:, None, :], (128, 16, 128))),
    }
    slopes = 2.0 ** (-(np.arange(16) + 1.0) / 2.0)
    msv = (slopes * np.sqrt(128.0)).astype(f32)
    hi = _bf16_round(msv)
    lo = _bf16_round(msv - hi)
    shared["ms"] = msv.reshape(1, 16)
    shared["alibr"] = np.stack([np.repeat(128 * hi, 128), np.repeat(hi, 128), np.repeat(128 * lo, 128),
                                np.repeat(lo, 128)]).astype(f32)
    pt = A(page_table).astype(np.int32) % NPOOL
    sc_all = A(state_conv)[0]
    in_maps = []
    p_ = np.arange(128)
    for core in range(8):
        g, c = core // 4, core % 4
        xin = np.zeros((NT, 128, D), f32)
        for i in range(8):
            j = 4 * i + c
            xin[i] = x_prompt[g, 128 * j:128 * j + 128]
            if j > 0:
                xin[8, 80 + 2 * i:82 + 2 * i] = x_prompt[g, 128 * j - 2:128 * j]
            else:
                xin[8, 80 + 2 * i:82 + 2 * i] = meta_tokens[14:16]
        xin[8, 0:16] = meta_tokens
        xin[8, 16:80] = x_sample[16 * core:16 * core + 16].reshape(64, D)
        xT = np.ascontiguousarray(xin.reshape(T, D).T.reshape(16, 128, T).transpose(1, 0, 2))
        cm = np.full((128, 4, 128), -1e30, f32)
        for dl in range(4):
            if dl < c:
                cm[:, dl, :] = 0.0
            elif dl == c:
                cm[:, dl, :] = np.where(p_[None, :] <= p_[:, None], 0.0, -1e30)
        al = np.zeros((5, 57, 128), f32)
        al[0] = 1.0
        for e in range(57):
            if e < 8:
                a_, b_ = -(4 * e + c + 1), np.where(p_ < 16, p_ - 15, 0)
            elif e < 40:
                a_, b_ = (e - 8 - 28) - c, p_ - 127
            elif e < 56:
                a_, b_ = (e - 40) - 16, p_ - 3
            else:
                a_, b_ = 0, np.where(p_ < 4, p_ - 3, 0)
            al[1, e] = a_; al[3, e] = a_; al[2, e] = b_; al[4, e] = b_
        t0 = np.array([143 + 128 * (4 * i + c) for i in range(8)] + [2051], f32).reshape(1, 9)
        sc = sc_all[16 * core:16 * core + 16]
        m = dict(shared)
        m.update({
            "xin": xin, "xTin": xT, "cmask": cm.reshape(128, 512), "alibl": al, "t0tab": t0,
            "sconv": np.ascontiguousarray(sc.reshape(16, 2, 16, 128).transpose(3, 2, 0, 1)),
            "ptab": np.ascontiguousarray(pt[16 * core:16 * core + 16].reshape(1, 256)),
        })
        in_maps.append(m)
    if "nc" not in _NC_CACHE:
        _NC_CACHE["nc"] = build_program()
    res = run_bass_kernel_spmd(_NC_CACHE["nc"], in_maps[:KCORES], core_ids=list(range(KCORES)))
    R = [{k: np.asarray(v) for k, v in r.items()} for r in res.results]
    B, S = 2, 4096
    y_prompt = np.zeros((B, S, D), f32); y_sample = np.zeros((128, 4, D), f32)
    nkp = np.zeros((1, B, S + 16, 4, 128), f32); nvp = np.zeros_like(nkp); nip = np.zeros((1, B, S + 16, 64), f32)
    ncp = np.zeros((1, B, 2, D), f32)
    nks = np.zeros((1, 128, 4, 4, 128), f32); nvs = np.zeros_like(nks); nis = np.zeros((1, 128, 4, 64), f32)
    ncs = np.zeros((1, 128, 2, D), f32)
    for core in range(KCORES):
        g, c = core // 4, core % 4
        r = R[core]
        for i in range(8):
            j = 4 * i + c
            y_prompt[g, 128 * j:128 * j + 128] = r["y_blk"][i]
            nkp[0, g, 16 + 128 * j:144 + 128 * j] = r["k_blk"][i].reshape(128, 4, 128)
            nvp[0, g, 16 + 128 * j:144 + 128 * j] = r["v_blk"][i].reshape(128, 4, 128)
            nip[0, g, 16 + 128 * j:144 + 128 * j] = r["ik_blk"][i]
        if c == 0:
            nkp[0, g, 0:16] = r["k_misc"][0:16].reshape(16, 4, 128)
            nvp[0, g, 0:16] = r["v_misc"][0:16].reshape(16, 4, 128)
            nip[0, g, 0:16] = r["ik_misc"][0:16]
        if c == 3:
            ncp[0, g] = r["ustate"][32:34]
        sl = slice(16 * core, 16 * core + 16)
        y_sample[sl] = r["y_misc"][16:80].reshape(16, 4, D)
        nks[0, sl] = r["k_misc"][16:80].reshape(16, 4, 4, 128)
        nvs[0, sl] = r["v_misc"][16:80].reshape(16, 4, 4, 128)
        nis[0, sl] = r["ik_misc"][16:80].reshape(16, 4, 64)
        ncs[0, sl] = r["ustate"][0:32].reshape(16, 2, D)
    return (y_prompt, y_sample, nkp, nvp, nip, ncp, nks, nvs, nis, ncs)
```

```python
import numpy as np
from contextlib import ExitStack
import concourse.bass as bass
import concourse.mybir as mybir
from concourse.bass_utils import run_bass_kernel_spmd

F32, BF16, I32 = mybir.dt.float32, mybir.dt.bfloat16, mybir.dt.int32
ALU = mybir.AluOpType
AF = mybir.ActivationFunctionType
AX = mybir.AxisListType


class Tok:
    __slots__ = ("w", "r", "name")

    def __init__(self, name=""):
        self.w = None
        self.r = {}
        self.name = name


class Eng:
    def __init__(self, K, name, same_raw):
        self.K, self.name, self.same_raw = K, name, same_raw
        self.sem = K.new_sem(name)
        self.cnt = 0
        self.waited = {}
        self.prog = []


class Kern:
    EPOCH = 30000

    def __init__(self, nc, es):
        self.nc, self.es = nc, es
        self.nsem = 0
        self.PE = Eng(self, "pe", False)
        self.ACT = Eng(self, "act", True)
        self.DVE = Eng(self, "dve", True)
        self.POOL = Eng(self, "pool", True)
        self.SP = Eng(self, "sp", True)
        self.rings = {}
        for q, n in (("sp", 20), ("pool", 6)):
            self.rings[q] = dict(sems=[self.new_sem(f"d{q}{i}") for i in range(n)], vals=[0] * n, k=0)
        self.out_events = []

    def new_sem(self, name):
        self.nsem += 1
        return self.es.enter_context(self.nc.semaphore(f"{name}_{self.nsem}"))

    def _waits(self, eng, rd, wr):
        deps = []
        for t in rd:
            if t.w is not None:
                deps.append(t.w)
        for t in wr:
            if t.w is not None:
                deps.append(t.w)
            deps.extend(t.r.values())
        for (sem, val, en) in deps:
            if en == eng.name and not eng.same_raw:
                continue
            key = id(sem)
            if eng.waited.get(key, 0) >= val:
                continue
            eng.waited[key] = val
            eng.prog.append(lambda e, sem=sem, val=val: e.wait_ge(sem, val))

    def _mark(self, ev, rd, wr):
        for t in rd:
            t.r[id(ev[0])] = ev
        for t in wr:
            t.w = ev
            t.r = {}

    def op(self, eng, fn, rd=(), wr=(), inc=True):
        self._waits(eng, rd, wr)
        if eng.cnt >= self.EPOCH and inc:
            pass
        ev = (eng.sem, eng.cnt + 1, eng.name)
        if inc:
            sem = eng.sem
            eng.prog.append(lambda e, fn=fn, sem=sem: fn(e).then_inc(sem, 1))
            eng.cnt += 1
            if eng.cnt >= self.EPOCH:
                eng.sem = self.new_sem(eng.name)
                eng.cnt = 0
        else:
            eng.prog.append(lambda e, fn=fn: fn(e))
        self._mark(ev, rd, wr)
        return ev

    def dma(self, eng, out, in_, rd=(), wr=(), fn=None, is_out=False):
        ring = self.rings[eng.name]
        i = ring["k"] % len(ring["sems"])
        ring["k"] += 1
        sem, prev = ring["sems"][i], ring["vals"][i]
        self._waits(eng, rd, wr)
        key = id(sem)
        if prev > 0 and eng.waited.get(key, 0) < prev:
            eng.waited[key] = prev
            eng.prog.append(lambda e, sem=sem, prev=prev: e.wait_ge(sem, prev))
        if fn is None:
            fn = lambda e, out=out, in_=in_: e.dma_start(out=out, in_=in_)
        eng.prog.append(lambda e, fn=fn, sem=sem: fn(e).then_inc(sem, 16))
        ring["vals"][i] = prev + 16
        ev = (sem, prev + 16, "dma")
        self._mark(ev, rd, wr)
        if is_out:
            self.out_events.append(ev)
        return ev

    def wait_event(self, eng, ev):
        sem, val, _ = ev
        if eng.waited.get(id(sem), 0) < val:
            eng.waited[id(sem)] = val
            eng.prog.append(lambda e, sem=sem, val=val: e.wait_ge(sem, val))

    def mm(self, out, lhsT, rhs, start, stop, rd, wr):
        return self.op(self.PE, lambda e: e.matmul(out, lhsT, rhs, start=start, stop=stop), rd, wr, inc=stop)

    def tr(self, out, in_, ident, rd, wr, inc=True):
        return self.op(self.PE, lambda e: e.transpose(out, in_, ident), rd, wr, inc=inc)

    def act(self, out, in_, func, rd, wr, bias=None, scale=None, accum_out=None, eng=None):
        kw = {}
        if bias is not None:
            kw["bias"] = bias
        if scale is not None:
            kw["scale"] = scale
        if accum_out is not None:
            kw["accum_out"] = accum_out
        return self.op(eng or self.ACT, lambda e: e.activation(out=out, in_=in_, func=func, **kw), rd, wr)

    def tt(self, out, in0, in1, op, rd, wr, eng=None):
        return self.op(eng or self.DVE, lambda e: e.tensor_tensor(out=out, in0=in0, in1=in1, op=op), rd, wr)

    def ts(self, out, in0, s1, s2, op0, op1=None, rd=(), wr=(), accum_out=None, eng=None):
        kw = {}
        if op1 is not None:
            kw["op1"] = op1
        if accum_out is not None:
            kw["accum_out"] = accum_out
        return self.op(eng or self.DVE,
                       lambda e: e.tensor_scalar(out=out, in0=in0, scalar1=s1, scalar2=s2, op0=op0, **kw), rd, wr)

    def stt(self, out, in0, scalar, in1, op0, op1, rd, wr, eng=None):
        return self.op(eng or self.DVE,
                       lambda e: e.scalar_tensor_tensor(out=out, in0=in0, scalar=scalar, in1=in1, op0=op0, op1=op1),
                       rd, wr)

    def copy(self, out, in_, rd, wr, eng=None):
        return self.op(eng or self.DVE, lambda e: e.tensor_copy(out=out, in_=in_), rd, wr)

    def red(self, out, in_, op, rd, wr, eng=None):
        return self.op(eng or self.DVE, lambda e: e.tensor_reduce(out=out, in_=in_, axis=AX.X, op=op), rd, wr)

    def memset(self, ap, val, wr, eng=None):
        return self.op(eng or self.DVE, lambda e: e.memset(ap, val), (), wr)

    def finish(self):
        nc = self.nc
        for ev in self.out_events:
            self.wait_event(self.SP, ev)
        with nc.Block() as block:
            @block.tensor
            def _(e):
                for th in self.PE.prog:
                    th(e)

            @block.scalar
            def _(e):
                for th in self.ACT.prog:
                    th(e)

            @block.vector
            def _(e):
                for th in self.DVE.prog:
                    th(e)

            @block.gpsimd
            def _(e):
                for th in self.POOL.prog:
                    th(e)

            @block.sync
            def _(e):
                for th in self.SP.prog:
                    th(e)

    def coll(self, kind, ins, outs, groups, rd, wr):
        eng = self.POOL
        self._waits(eng, rd, wr)
        sem = self.new_sem("cc")
        eng.prog.append(lambda e: e.collective_compute(kind, ALU.bypass, replica_groups=groups,
                                                       ins=ins, outs=outs).then_inc(sem))
        ev = (sem, 1, "coll")
        self._mark(ev, rd, wr)
        return ev


D = 2048
T = 1152
NT = 9
TG = [(0, 512), (512, 512), (1024, 128)]
DFF = 5632
NU = 22
ALPHA = 2.0 ** 0.25
EPS = 1e-5
C0 = 25.0
SCALE = 128.0 ** -0.5
NITER = 20
import os
NPOOL = int(os.environ.get('KNPOOL', '2560'))
KSTOP = os.environ.get('KSTOP', '')
KCORES = int(os.environ.get('KCORES', '8'))


class _Stop(Exception):
    pass
FM_GB, FM_GC, FM_H, FM_Q, FM_KT, FM_IQ, FM_IK, FM_GCONV, FM_GATTN, NFM = 0, 16, 32, 48, 64, 68, 76, 77, 93, 109
NENT = 57


def build_program():
    nc = bass.Bass("TRN2", target_bir_lowering=False)
    es = ExitStack()
    din = lambda n, s, d=F32: nc.dram_tensor(n, list(s), d, kind="ExternalInput").ap()
    dout = lambda n, s, d=F32: nc.dram_tensor(n, list(s), d, kind="ExternalOutput").ap()
    dscr = lambda n, s, d: nc.dram_tensor(n, list(s), d)
    xin = din("xin", [NT, 128, D]); xTin = din("xTin", [128, 16, T])
    wgu = [din("wgu1", [NU, 128, 16, 2, 256]), din("wgu2", [NU, 128, 16, 2, 256])]
    wdn = [din("wd1", [NU, 128, 2, D]), din("wd2", [NU, 128, 2, D])]
    wfm = din("wfm", [NFM, 128, 16, 128])
    wk_d = din("wk", [128, 16, 512]); wv_d = din("wv", [128, 16, 512]); wikw_d = din("wikw", [128, 16, 80])
    wco = din("wco", [16, 128, 16, 128]); wao = din("wao", [16, 128, 16, 128]); wo_d = din("wo", [8, 128, 16, 256])
    lnp = din("lnp", [6, D]); convw_d = din("convw", [128, 16, 3]); sconv_d = din("sconv", [128, 16, 16, 2])
    ck_d = din("ck", [NPOOL * 128, 512]); cv_d = din("cv", [NPOOL * 128, 512]); cik_d = din("cik", [NPOOL * 128, 64])
    ptab_d = din("ptab", [1, 256], I32); iota_d = din("iota", [128, 1], I32)
    ident_d = din("ident", [128, 128]); cmask_d = din("cmask", [128, 512]); cms_d = din("cms", [4, 4])
    sel_d = din("sel", [64, 16]); cms64_d = din("cms64", [64, 4])
    alibl_d = din("alibl", [5, NENT, 128]); alibr_d = din("alibr", [4, 2048]); pow2_d = din("pow2", [1, NITER + 1])
    ms_d = din("ms", [1, 16]); t0_d = din("t0tab", [1, 9]); kpos_d = din("kpos", [1, 4112]); idt16_d = din("idt16", [128, 16, 128])
    y_blk = dout("y_blk", [8, 128, D]); y_misc = dout("y_misc", [128, D])
    k_blk = dout("k_blk", [8, 128, 512]); k_misc = dout("k_misc", [128, 512])
    v_blk = dout("v_blk", [8, 128, 512]); v_misc = dout("v_misc", [128, 512])
    ik_blk = dout("ik_blk", [8, 128, 64]); ik_misc = dout("ik_misc", [128, 64])
    ust_o = dout("ustate", [34, D])
    X1s = dscr("X1s", [NT, 128, D], F32); XTs = dscr("XTs", [128, 16, T], BF16)
    qTs = dscr("qTs", [16, 128, T], BF16); iqTs = dscr("iqTs", [8, 128, T], BF16)
    mcTs = dscr("mcTs", [16, 128, T], BF16); oTs = dscr("oTs", [16, 128, T], BF16)
    agki = dscr("agki", [512, 1024], BF16); agko = dscr("agko", [2048, 1024], BF16)
    agvi = dscr("agvi", [1024, 512], BF16); agvo = dscr("agvo", [4096, 512], BF16)
    agii = dscr("agii", [128, 1024], BF16); agio = dscr("agio", [512, 1024], BF16)
    vns = dscr("vns", [128, 512], BF16)
    sb = lambda n, s, d: es.enter_context(nc.sbuf_tensor(n, list(s), d))
    try:
        with es:
            K = Kern(nc, es)
            PE, ACTE, DVE, POOL, SP = K.PE, K.ACT, K.DVE, K.POOL, K.SP

            def stop(tag):
                if KSTOP == tag:
                    print("STOP", tag, {e.name: (e.cnt, len(e.prog)) for e in (PE, ACTE, DVE, POOL, SP)}, flush=True)
                    K.finish()
                    raise _Stop()
            FB = [es.enter_context(nc.psum_tensor(f"fb{i}", [128, 512], F32)) for i in range(6)]
            tFB = [Tok() for _ in range(6)]
            BB = [es.enter_context(nc.psum_tensor(f"bb{i}", [128, 1024], BF16)) for i in range(2)]
            tBB = [Tok() for _ in range(2)]
            st = dict(fa=0, bb=0)

            def fbank(lo=0, hi=6):
                i = lo + st["fa"] % (hi - lo)
                st["fa"] += 1
                return FB[i], tFB[i]

            def bbank():
                i = st["bb"] % 2
                st["bb"] += 1
                return BB[i], tBB[i]

            IDb = sb("IDb", [128, 128], BF16); tIDb = Tok()
            ONESb = sb("ONESb", [128, 128], BF16); tONES = Tok()
            STAT = sb("STAT", [128, 8, NT], F32); tSTAT = Tok()
            WQ = sb("WQ", [128, NT, 16], F32); tWQ = Tok()
            K.dma(POOL, IDb[:], ident_d[:, :], wr=[tIDb])
            K.memset(ONESb[:], 1.0, wr=[tONES])
            tXs, tXTs, tqTs, tiqTs, tmcTs, toTs = Tok(), Tok(), Tok(), Tok(), Tok(), Tok()

            def ffn(X, tX, XT, tXT, fi, scope):
                WGU = [scope("WGU0", [128, 16, 2, 256], BF16), scope("WGU1", [128, 16, 2, 256], BF16)]
                tWGU = [Tok(), Tok()]
                WD = scope("WD", [128, 4, D], BF16); tWD = Tok()
                HT = scope("HT", [128, 4, T], BF16); tHT = [[Tok() for _ in TG] for _ in range(4)]
                SG = [scope("SG0", [128, 512], F32), scope("SG1", [128, 512], F32)]; tSG = [Tok(), Tok()]
                n = 0
                for up in range(NU // 2):
                    for uu in range(2):
                        u = 2 * up + uu
                        wg, tw = WGU[u % 2], tWGU[u % 2]
                        K.dma(POOL, wg[:], wgu[fi][u], wr=[tw])
                        K.dma(POOL, WD[:, 2 * uu:2 * uu + 2, :], wdn[fi][u], wr=[tWD])
                        for fc in range(2):
                            f4 = 2 * uu + fc
                            for gi, (g0, gn) in enumerate(TG):
                                pg, tpg = fbank(0, 4)
                                for kc in range(16):
                                    K.mm(pg[:, :gn], wg[:, kc, 0, fc * 128:(fc + 1) * 128], XT[:, kc, g0:g0 + gn],
                                         kc == 0, kc == 15, [tw, tXT], [tpg])
                                pu, tpu = fbank(0, 4)
                                for kc in range(16):
                                    K.mm(pu[:, :gn], wg[:, kc, 1, fc * 128:(fc + 1) * 128], XT[:, kc, g0:g0 + gn],
                                         kc == 0, kc == 15, [tw, tXT], [tpu])
                                sg, tsg = SG[n % 2], tSG[n % 2]
                                n += 1
                                K.act(sg[:, :gn], pg[:, :gn], AF.Silu, [tpg], [tsg])
                                K.tt(HT[:, f4, g0:g0 + gn], sg[:, :gn], pu[:, :gn], ALU.mult, [tsg, tpu], [tHT[f4][gi]])
                    for t in range(NT):
                        gi = min(t // 4, 2)
                        for dq in range(4):
                            po, tpo = fbank(4, 6)
                            for f4 in range(4):
                                K.mm(po[:, :], HT[:, f4, t * 128:(t + 1) * 128], WD[:, f4, dq * 512:(dq + 1) * 512],
                                     f4 == 0, f4 == 3, [tHT[f4][gi], tWD], [tpo])
                            xs = X[:, t, dq * 512:(dq + 1) * 512]
                            K.stt(xs, po[:, :], 0.5, xs, ALU.mult, ALU.add, [tpo, tX[t][dq]], [tX[t][dq]])

            def ln_tiles(X, tX, li, scope):
                SQ = scope("SQ", [128, D], F32); tSQ = Tok()
                LNG = scope("LNG", [128, D], F32); LNB = scope("LNB", [128, D], F32); tLNG, tLNB = Tok(), Tok()
                K.dma(SP, LNG[:], lnp[2 * li:2 * li + 1, :].partition_broadcast(128), wr=[tLNG])
                K.dma(SP, LNB[:], lnp[2 * li + 1:2 * li + 2, :].partition_broadcast(128), wr=[tLNB])
                for t in range(NT):
                    K.red(STAT[:, 0, t:t + 1], X[:, t, :], ALU.add, tX[t], [tSTAT])
                    K.act(SQ[:], X[:, t, :], AF.Square, tX[t], [tSQ])
                    K.red(STAT[:, 1, t:t + 1], SQ[:], ALU.add, [tSQ], [tSTAT])
                S = lambda r: STAT[:, r, :]
                K.ts(S(2), S(0), 1.0 / D, None, ALU.mult, rd=[tSTAT], wr=[tSTAT])
                K.ts(S(3), S(1), 1.0 / D, None, ALU.mult, rd=[tSTAT], wr=[tSTAT])
                K.tt(S(4), S(2), S(2), ALU.mult, [tSTAT], [tSTAT])
                K.tt(S(3), S(3), S(4), ALU.subtract, [tSTAT], [tSTAT])
                K.ts(S(3), S(3), EPS, None, ALU.add, rd=[tSTAT], wr=[tSTAT])
                K.act(S(4), S(3), AF.Sqrt, [tSTAT], [tSTAT])
                K.op(DVE, lambda e: e.reciprocal(out=S(5), in_=S(4)), [tSTAT], [tSTAT])
                K.stt(S(6), S(2), -1.0, S(5), ALU.mult, ALU.mult, [tSTAT], [tSTAT])
                for t in range(NT):
                    K.act(X[:, t, :], X[:, t, :], AF.Identity, [tSTAT] + tX[t], tX[t],
                          bias=STAT[:, 6, t:t + 1], scale=STAT[:, 5, t:t + 1])
                    K.tt(X[:, t, :], X[:, t, :], LNG[:], ALU.mult, [tLNG] + tX[t], tX[t])
                    K.tt(X[:, t, :], X[:, t, :], LNB[:], ALU.add, [tLNB] + tX[t], tX[t])

            def to_fm(X, tX, XT, tXT, scope):
                XB = [scope("XB0", [128, D], BF16), scope("XB1", [128, D], BF16)]; tXB = [Tok(), Tok()]
                for t in range(NT):
                    xb, txb = XB[t % 2], tXB[t % 2]
                    K.act(xb[:], X[:, t, :], AF.Copy, tX[t], [txb])
                    for half in range(2):
                        pb, tpb = bbank()
                        for j in range(8):
                            kc = half * 8 + j
                            K.tr(pb[:, j * 128:(j + 1) * 128], xb[:, kc * 128:(kc + 1) * 128], IDb[:], [txb, tIDb], [tpb],
                                 inc=(j == 7))
                        K.copy(XT[:, half * 8:half * 8 + 8, t * 128:(t + 1) * 128],
                               pb[:, :].rearrange("p (j q) -> p j q", j=8), [tpb], [tXT])

            def scoped():
                s = ExitStack()
                st["sc"] = st.get("sc", 0) + 1
                sid = st["sc"]
                return s, (lambda n, shp, d: s.enter_context(nc.sbuf_tensor(f"{n}_s{sid}", list(shp), d)))

            s23 = es.enter_context(ExitStack())
            sb23 = lambda n, shp, d: s23.enter_context(nc.sbuf_tensor(n, list(shp), d))
            KTO = sb23("KTO", [128, 4, T], BF16); tKTO = Tok()
            IKTO = sb23("IKTO", [128, T], BF16); tIKTO = Tok()
            VMISC = sb23("VMISC", [128, 512], BF16); tVMISC = Tok()
            sxt = es.enter_context(ExitStack())
            XT = sxt.enter_context(nc.sbuf_tensor("XT", [128, 16, T], BF16)); tXT = Tok()
            tX = [[Tok() for _ in range(4)] for _ in range(NT)]

            def fence():
                evs = []
                for e in (PE, ACTE, DVE):
                    if e.cnt > 0:
                        evs.append((e.sem, e.cnt, e.name))
                for q in K.rings.values():
                    for s_, v_ in zip(q["sems"], q["vals"]):
                        if v_ > 0:
                            evs.append((s_, v_, "dma"))
                for e in (PE, ACTE, DVE, POOL, SP):
                    for ev in evs:
                        if ev[2] == e.name and not e.same_raw:
                            continue
                        K.wait_event(e, ev)

            s1, sc1 = scoped()
            with s1:
                X = sc1("X", [128, NT, D], F32)
                K.dma(POOL, XT[:], xTin[:, :, :], wr=[tXT])
                for t in range(NT):
                    K.dma(SP, X[:, t, :], xin[t], wr=tX[t])
                    K.act(X[:, t, :], X[:, t, :], AF.Identity, tX[t], tX[t], scale=ALPHA)
                sf, scf = scoped()
                with sf:
                    ffn(X, tX, XT, tXT, 0, scf)
                fence()
                sl, scl = scoped()
                with sl:
                    ln_tiles(X, tX, 0, scl)
                    to_fm(X, tX, XT, tXT, scl)
                fence()
                for t in range(NT):
                    K.dma(SP, X1s[t], X[:, t, :], rd=tX[t], wr=[tXs])
                K.dma(SP, XTs[:, :, :], XT[:], rd=[tXT], wr=[tXTs])
                tS1 = tX

            fence()

            stop("1")
            s2, sc2 = scoped()
            with s2:
                WF = [sc2(f"WF{i}", [128, 16, 128], BF16) for i in range(6)]; tWF = [Tok() for _ in range(6)]
                wst = dict(k=0)

                def load_w(src):
                    i = wst["k"] % 6
                    wst["k"] += 1
                    K.dma(POOL, WF[i][:], src, wr=[tWF[i]])
                    return WF[i], tWF[i]

                def proj(w, tw, SRC, tSRC, gi):
                    g0, gn = TG[gi]
                    pb, tp = fbank()
                    for kc in range(16):
                        K.mm(pb[:, :gn], w[:, kc, :], SRC[:, kc, g0:g0 + gn], kc == 0, kc == 15, [tw, tSRC], [tp])
                    return pb, tp

                ZT = sc2("ZT", [128, 16, T], BF16); tZT = Tok()
                for cc0 in range(16):
                    K.memset(ZT[:, cc0, :], 0.0, wr=[tZT], eng=DVE)
                CONVW = sc2("CONVW", [128, 16, 3], F32); tCW = Tok()
                SCONV = sc2("SCONV", [128, 16, 16, 2], F32); tSC = Tok()
                K.dma(SP, CONVW[:], convw_d[:, :, :], wr=[tCW])
                K.dma(SP, SCONV[:], sconv_d[:, :, :, :], wr=[tSC])
                GC = sc2("GC", [128, T], F32); tGC = Tok()
                GB = sc2("GB", [128, T], F32); tGB = Tok()
                UE = sc2("UE", [128, 8, 130], F32); US = sc2("US", [128, 16, 6], F32); UM = sc2("UM", [128, 18], F32)
                tU = Tok()
                CVB = sc2("CVB", [128, 8, 128], F32); CVS = sc2("CVS", [128, 16, 4], F32); CVM = sc2("CVM", [128, 16], F32)
                tCV = Tok()
                UTM_M = sc2("UTM_M", [128, D], F32); UTM_7 = sc2("UTM_7", [128, D], F32); tUTM, tUT7 = Tok(), Tok()
                TMPG = sc2("TMPG", [128, 128], F32); tTMPG = Tok()
                K.memset(UM[:], 0.0, wr=[tU])
                for cc in range(16):
                    wgc, twgc = load_w(wfm[FM_GC + cc]); wh, twh = load_w(wfm[FM_H + cc]); wgb, twgb = load_w(wfm[FM_GB + cc])
                    for gi in range(3):
                        g0, gn = TG[gi]
                        pb, tp = proj(wgc, twgc, XT, tXT, gi)
                        K.act(GC[:, g0:g0 + gn], pb[:, :gn], AF.Copy, [tp], [tGC])
                    for gi in range(3):
                        g0, gn = TG[gi]
                        pb, tp = proj(wh, twh, XT, tXT, gi)
                        if gi < 2:
                            K.tt(UE[:, 4 * gi:4 * gi + 4, 2:130], GC[:, g0:g0 + 512].rearrange("p (b q) -> p b q", b=4),
                                 pb[:, :].rearrange("p (b q) -> p b q", b=4), ALU.mult, [tp, tGC], [tU])
                        else:
                            K.tt(UM[:, 2:18], GC[:, 1024:1040], pb[:, 0:16], ALU.mult, [tp, tGC], [tU])
                            K.tt(US[:, :, 2:6], GC[:, 1040:1104].rearrange("p (s q) -> p s q", q=4),
                                 pb[:, 16:80].rearrange("p (s q) -> p s q", q=4), ALU.mult, [tp, tGC], [tU])
                            K.tt(UE[:, :, 0:2], GC[:, 1104:1120].rearrange("p (b q) -> p b q", q=2),
                                 pb[:, 80:96].rearrange("p (b q) -> p b q", q=2), ALU.mult, [tp, tGC], [tU])
                    K.copy(US[:, :, 0:2], SCONV[:, cc, :, :], [tSC], [tU])
                    for gi in range(3):
                        g0, gn = TG[gi]
                        pb, tp = proj(wgb, twgb, XT, tXT, gi)
                        K.act(GB[:, g0:g0 + gn], pb[:, :gn], AF.Copy, [tp], [tGB])
                    w0, w1, w2 = (CONVW[:, cc, j:j + 1] for j in range(3))
                    for (cv, src, a, wdt) in ((CVB[:], lambda o: UE[:, :, o:o + 128], 0, 128),
                                              (CVS[:], lambda o: US[:, :, o:o + 4], 0, 4),
                                              (CVM[:], lambda o: UM[:, o:o + 16], 0, 16)):
                        K.ts(cv, src(2), w2, None, ALU.mult, rd=[tU, tCW], wr=[tCV])
                        K.stt(cv, src(1), w1, cv, ALU.mult, ALU.add, [tU, tCW, tCV], [tCV])
                        K.stt(cv, src(0), w0, cv, ALU.mult, ALU.add, [tU, tCW, tCV], [tCV])
                    K.tt(ZT[:, cc, 0:1024].rearrange("p (b q) -> p b q", b=8), GB[:, 0:1024].rearrange("p (b q) -> p b q", b=8),
                         CVB[:], ALU.mult, [tGB, tCV], [tZT])
                    K.tt(ZT[:, cc, 1024:1040], GB[:, 1024:1040], CVM[:], ALU.mult, [tGB, tCV], [tZT])
                    K.tt(ZT[:, cc, 1040:1104].rearrange("p (s q) -> p s q", q=4), GB[:, 1040:1104].rearrange("p (s q) -> p s q", q=4),
                         CVS[:], ALU.mult, [tGB, tCV], [tZT])
                    for (ti, UT, tUT) in ((8, UTM_M, tUTM), (7, UTM_7, tUT7)):
                        pg2, tpg2 = fbank()
                        for kc in range(16):
                            K.mm(pg2[:, 0:128], XT[:, kc, ti * 128:(ti + 1) * 128], wgc[:, kc, :], kc == 0, kc == 15, [tXT, twgc], [tpg2])
                        ph2, tph2 = fbank()
                        for kc in range(16):
                            K.mm(ph2[:, 0:128], XT[:, kc, ti * 128:(ti + 1) * 128], wh[:, kc, :], kc == 0, kc == 15, [tXT, twh], [tph2])
                        K.act(TMPG[:, :], pg2[:, 0:128], AF.Copy, [tpg2], [tTMPG])
                        K.tt(UT[:, cc * 128:(cc + 1) * 128], TMPG[:, :], ph2[:, 0:128], ALU.mult, [tTMPG, tph2], [tUT])
                for s_i in range(16):
                    K.dma(SP, ust_o[2 * s_i:2 * s_i + 2, :], UTM_M[18 + 4 * s_i:20 + 4 * s_i, :], rd=[tUTM], is_out=True)
                K.dma(SP, ust_o[32:34, :], UTM_7[126:128, :], rd=[tUT7], is_out=True)
                stop("2a")
                SGT = [sc2("SGT0", [128, 512], F32), sc2("SGT1", [128, 512], F32)]; tSGT = [Tok(), Tok()]
                MCC = [sc2("MCC0", [128, T], BF16), sc2("MCC1", [128, T], BF16)]; tMCC = [Tok(), Tok()]
                n = 0
                for oc in range(16):
                    wc_, twc = load_w(wco[oc]); wg_, twg = load_w(wfm[FM_GCONV + oc])
                    mcc, tmcc = MCC[oc % 2], tMCC[oc % 2]
                    for gi in range(3):
                        g0, gn = TG[gi]
                        pg, tpg = proj(wg_, twg, XT, tXT, gi)
                        py, tpy = proj(wc_, twc, ZT, tZT, gi)
                        sg, tsg = SGT[n % 2], tSGT[n % 2]; n += 1
                        K.act(sg[:, :gn], pg[:, :gn], AF.Sigmoid, [tpg], [tsg])
                        K.tt(mcc[:, g0:g0 + gn], sg[:, :gn], py[:, :gn], ALU.mult, [tsg, tpy], [tmcc])
                    K.dma(SP, mcTs[oc], mcc[:], rd=[tmcc], wr=[tmcTs])
                stop("2b")
                for (base, cnt, dst, tdst) in ((FM_Q, 16, qTs, tqTs), (FM_IQ, 8, iqTs, tiqTs)):
                    for h in range(cnt):
                        w_, tw_ = load_w(wfm[base + h])
                        mcc, tmcc = MCC[h % 2], tMCC[h % 2]
                        for gi in range(3):
                            g0, gn = TG[gi]
                            pb, tp = proj(w_, tw_, XT, tXT, gi)
                            K.act(mcc[:, g0:g0 + gn], pb[:, :gn], AF.Copy, [tp], [tmcc])
                        K.dma(SP, dst[h], mcc[:], rd=[tmcc], wr=[tdst])
            fence()
            stop("2c")
            s2b, sc2b = scoped()
            tagki, tagvi, tagii, tagko, tagvo, tagio = (Tok() for _ in range(6))
            with s2b:
                WF2 = [sc2b(f"WG{i}", [128, 16, 128], BF16) for i in range(2)]; tWF2 = [Tok(), Tok()]
                for g in range(5):
                    w_, tw_ = WF2[g % 2], tWF2[g % 2]
                    K.dma(POOL, w_[:], wfm[FM_KT + g] if g < 4 else wfm[FM_IK], wr=[tw_])
                    for gi in range(3):
                        g0, gn = TG[gi]
                        pb, tp = fbank()
                        for kc in range(16):
                            K.mm(pb[:, :gn], w_[:, kc, :], XT[:, kc, g0:g0 + gn], kc == 0, kc == 15, [tw_, tXT], [tp])
                        if g < 4:
                            K.act(KTO[:, g, g0:g0 + gn], pb[:, :gn], AF.Copy, [tp], [tKTO])
                        else:
                            K.act(IKTO[:, g0:g0 + gn], pb[:, :gn], AF.Copy, [tp], [tIKTO])
                stop("2c1")
                for g in range(4):
                    K.dma(SP, agki[g * 128:(g + 1) * 128, :], KTO[:, g, 0:1024], rd=[tKTO], wr=[tagki])
                stop("2c2")
                K.dma(SP, agii[:, :], IKTO[:, 0:1024], rd=[tIKTO], wr=[tagii])
                stop("2d")
                WK = sc2b("WK", [128, 16, 512], BF16); WV = sc2b("WV", [128, 16, 512], BF16); WI = sc2b("WI", [128, 16, 80], BF16)
                tWK, tWV, tWI = Tok(), Tok(), Tok()
                K.dma(POOL, WK[:], wk_d[:, :, :], wr=[tWK]); K.dma(POOL, WV[:], wv_d[:, :, :], wr=[tWV])
                K.dma(POOL, WI[:], wikw_d[:, :, :], wr=[tWI])
                KTM = [sc2b(f"KTM{i}", [128, 512], F32) for i in range(2)]; tKTM = [Tok(), Tok()]
                VTM = [sc2b(f"VTM{i}", [128, 512], F32) for i in range(2)]; tVTM = [Tok(), Tok()]
                VBF = [sc2b(f"VBF{i}", [128, 512], BF16) for i in range(2)]; tVBF = [Tok(), Tok()]
                ITM = [sc2b(f"ITM{i}", [128, 80], F32) for i in range(2)]; tITM = [Tok(), Tok()]
                for t in range(NT):
                    xs = lambda kc: XT[:, kc, t * 128:(t + 1) * 128]
                    i2 = t % 2
                    pk, tpk = fbank()
                    for kc in range(16):
                        K.mm(pk[:, :], xs(kc), WK[:, kc, :], kc == 0, kc == 15, [tXT, tWK], [tpk])
                    K.act(KTM[i2][:], pk[:, :], AF.Copy, [tpk], [tKTM[i2]])
                    K.dma(SP, k_blk[t] if t < 8 else k_misc[:, :], KTM[i2][:], rd=[tKTM[i2]], is_out=True)
                    stop(f"t{t}k")
                    pv, tpv = fbank()
                    for kc in range(16):
                        K.mm(pv[:, :], xs(kc), WV[:, kc, :], kc == 0, kc == 15, [tXT, tWV], [tpv])
                    K.act(VTM[i2][:], pv[:, :], AF.Copy, [tpv], [tVTM[i2]])
                    K.dma(SP, v_blk[t] if t < 8 else v_misc[:, :], VTM[i2][:], rd=[tVTM[i2]], is_out=True)
                    if t < 8:
                        K.copy(VBF[i2][:], VTM[i2][:], [tVTM[i2]], [tVBF[i2]])
                        K.dma(SP, agvi[t * 128:(t + 1) * 128, :], VBF[i2][:], rd=[tVBF[i2]], wr=[tagvi])
                    else:
                        K.copy(VMISC[:], VTM[i2][:], [tVTM[i2]], [tVMISC])
                        K.dma(SP, vns[:, :], VMISC[:, :], rd=[tVMISC], wr=[tagvi])
                    stop(f"t{t}v")
                    pi, tpi = fbank()
                    for kc in range(16):
                        K.mm(pi[:, 0:80], xs(kc), WI[:, kc, :], kc == 0, kc == 15, [tXT, tWI], [tpi])
                    K.act(ITM[i2][:], pi[:, 0:80], AF.Copy, [tpi], [tITM[i2]])
                    K.dma(SP, ik_blk[t] if t < 8 else ik_misc[:, :], ITM[i2][:, 0:64], rd=[tITM[i2]], is_out=True)
                    stop(f"t{t}i")
                    K.ts(WQ[:, t, :], ITM[i2][:, 64:80], 1.0 / 32.0, None, ALU.mult, rd=[tITM[i2]], wr=[tWQ])
                    stop(f"t{t}w")
                stop("2e")
                grp = [[0, 1, 2, 3], [4, 5, 6, 7]]
                v128 = lambda h, p: h.ap().rearrange("(p a) c -> p (a c)", p=p).opt()
                if os.environ.get("KNOCOLL", "0") != "1":
                    K.coll("AllGather", [v128(agki, 128)], [v128(agko, 512)], grp, [tagki], [tagko])
                    K.coll("AllGather", [v128(agvi, 128)], [v128(agvo, 512)], grp, [tagvi], [tagvo])
                    K.coll("AllGather", [v128(agii, 128)], [v128(agio, 512)], grp, [tagii], [tagio])
            fence()
            stop("2")
            sxt.close()

            NENT2 = 57
            s3, sc3 = scoped()
            with s3:
                KT = sc3("KT", [128, 4, 4112], BF16); tKT = Tok()
                VA = sc3("VA", [128, 33, 512], BF16); tVA = Tok()
                IKT = sc3("IKT", [128, 4112], BF16); tIKT = Tok()
                I = sc3("I", [128, 4112], F32)
                MNEGT = sc3("MNEGT", [128, 33, 128], BF16); tMT = Tok()
                JUNK = sc3("JUNK", [128, 4112], BF16); tJ = Tok()
                MNEG, tMNEG = JUNK, tJ
                KPOS = sc3("KPOS", [128, 4112], BF16); tKPOS = Tok()
                QT = sc3("QT", [128, 16, 128], BF16); tQT = Tok()
                IQT = sc3("IQT", [128, 8, 128], BF16); tIQT = Tok()
                OTS = sc3("OTS", [128, 16, 128], BF16); tOTS = Tok()
                PT = [sc3(f"PT{i}", [128, 512], BF16) for i in range(2)]; tPT = [Tok(), Tok()]
                R = [sc3(f"R{i}", [128, 512], F32) for i in range(2)]; tR = [Tok(), Tok()]
                DIAGX = sc3("DIAGX", [128, 16, 128], BF16); tDX = Tok()
                IDT16 = sc3("IDT16", [128, 16, 128], BF16); tI16 = Tok()
                ALIBL = sc3("ALIBL", [5, NENT2, 128], BF16); tAL = Tok()
                ALIBR5 = sc3("ALIBR5", [5, 16, 128], BF16); tAR = Tok()
                CM = sc3("CM", [128, 512], F32); CMS = sc3("CMS", [4, 4], F32); tCM = Tok()
                POW2 = sc3("POW2", [128, NITER + 1], F32); MS = sc3("MS", [128, 16], F32); T0T = sc3("T0T", [128, 9], F32)
                tCST = Tok()
                SM = sc3("SM", [128, 8], F32); CNT = sc3("CNT", [128, NITER], F32); WTAB = sc3("WTAB", [128, NITER + 1], F32)
                RTM = sc3("RTM", [128, 16], F32); tSM = Tok()
                tRINV = Tok()
                tWQS = Tok()
                wqs_d = dscr("wqs_d", [64, 16], F32); twqsd = Tok()
                K.dma(POOL, KPOS[:], kpos_d[0:1, :].partition_broadcast(128), wr=[tKPOS])
                K.dma(POOL, IDT16[:], idt16_d[:, :, :], wr=[tI16])
                K.dma(POOL, ALIBL[:], alibl_d[:, :, :], wr=[tAL])
                K.dma(POOL, ALIBR5[1:5, :, :], alibr_d[:, :].rearrange("r (h q) -> r h q", h=16), wr=[tAR])
                K.dma(SP, CM[:], cmask_d[:, :], wr=[tCM]); K.dma(SP, CMS[:], cms_d[:, :], wr=[tCM])
                K.dma(SP, POW2[:], pow2_d[0:1, :].partition_broadcast(128), wr=[tCST])
                K.dma(SP, MS[:], ms_d[0:1, :].partition_broadcast(128), wr=[tCST])
                K.dma(SP, T0T[:], t0_d[0:1, :].partition_broadcast(128), wr=[tCST])
                K.dma(SP, wqs_d[:, :], WQ[16:80, 8, :], rd=[tWQ], wr=[twqsd])
                for g in range(4):
                    for r in range(4):
                        K.dma(SP, KT[:, g, 16:4112].rearrange("d (i r p) -> d i r p", r=4, p=128)[:, :, r, :],
                              agko[r * 512 + g * 128:r * 512 + (g + 1) * 128, :].rearrange("d (i p) -> d i p", p=128),
                              rd=[tagko], wr=[tKT])
                for r in range(4):
                    K.dma(SP, VA[:, 1:33, :].rearrange("p (i r) f -> p i r f", r=4)[:, :, r, :],
                          agvo[r * 1024:(r + 1) * 1024, :].rearrange("(i p) f -> p i f", p=128), rd=[tagvo], wr=[tVA])
                    K.dma(SP, IKT[:, 16:4112].rearrange("d (i r p) -> d i r p", r=4, p=128)[:, :, r, :],
                          agio[r * 128:(r + 1) * 128, :].rearrange("d (i p) -> d i p", p=128), rd=[tagio], wr=[tIKT])
                K.copy(KT[:, :, 0:16], KTO[:, :, 1024:1040], [tKTO], [tKT])
                K.copy(IKT[:, 0:16], IKTO[:, 1024:1040], [tIKTO], [tIKT])
                K.copy(VA[0:16, 0, :], VMISC[0:16, :], [tVMISC], [tVA])
                ALR = ALIBR5
                ACCS = sc3("ACCS", [128, 4, 512], BF16); RSS = sc3("RSS", [128, 4, 512], F32); tACCS = Tok()
                RINV = RSS[:, 0, :]
                QTb = sc3("QTb", [128, 16, 128], BF16); IQTb = sc3("IQTb", [128, 8, 128], BF16)
                MNEGTb = sc3("MNEGTb", [128, 33, 128], BF16); ALRb = sc3("ALRb", [5, 16, 128], BF16)
                K.dma(POOL, ALRb[1:5, :, :], alibr_d[:, :].rearrange("r (h q) -> r h q", h=16), wr=[tAR])
                CTX = [dict(QT=QT, tQT=tQT, IQT=IQT, tIQT=tIQT, MNEGT=MNEGT, tMT=tMT, ALR=ALIBR5, tAR=tAR),
                       dict(QT=QTb, tQT=Tok(), IQT=IQTb, tIQT=Tok(), MNEGT=MNEGTb, tMT=Tok(), ALR=ALRb, tAR=tAR)]

                v3g = lambda ap, rows, nq: ap[0:rows, :].rearrange("p (h q) -> p h q", h=4)[:, :, 0:nq]
                cnt = dict(n=0, m=0)

                def indexer(nq, q0, L, wq_of_h, tIr, first=True, cx=None):
                    cx = cx or CTX[0]
                    IQT, tIQT = cx['IQT'], cx['tIQT']
                    ranges = [(k0, min(512, L - k0)) for k0 in range(0, L, 512)]
                    for h in range(16):
                        for ri, (k0, kn) in enumerate(ranges):
                            e2, p = h % 2, h // 2
                            pb, tp = fbank(0, 4)
                            K.mm(pb[0:nq, 0:kn], IQT[64 * e2:64 * e2 + 64, p, q0:q0 + nq], IKT[64 * e2:64 * e2 + 64, k0:k0 + kn],
                                 True, True, [tIQT, tIKT], [tp])
                            r_, tr_ = R[cnt["n"] % 2], tR[cnt["n"] % 2]; cnt["n"] += 1
                            K.act(r_[0:nq, 0:kn], pb[0:nq, 0:kn], AF.Relu, [tp], [tr_])
                            if h == 0 and first:
                                K.ts(I[0:nq, k0:k0 + kn], r_[0:nq, 0:kn], wq_of_h(0), None, ALU.mult, rd=[tr_, tWQ, tWQS], wr=[tIr[ri]])
                            else:
                                K.stt(I[0:nq, k0:k0 + kn], r_[0:nq, 0:kn], wq_of_h(h), I[0:nq, k0:k0 + kn], ALU.mult, ALU.add,
                                      [tr_, tWQ, tWQS, tIr[ri]], [tIr[ri]])

                def select(nq, chunks, L, cm_ap, cm_w, t0col, tIr, cx=None, do_b=True):
                    cx = cx or CTX[0]
                    MNEGT, tMT, ALR, tAR = cx['MNEGT'], cx['tMT'], cx['ALR'], cx['tAR']
                    sm = lambda c: SM[0:nq, c:c + 1]
                    Iv = I[0:nq, 0:L]
                    K.red(sm(0), Iv, ALU.min, tIr, [tSM]); K.red(sm(1), Iv, ALU.max, tIr, [tSM])
                    K.tt(I[0:nq, L - cm_w:L], I[0:nq, L - cm_w:L], cm_ap, ALU.add, [tCM] + tIr, tIr)
                    K.tt(sm(2), sm(1), sm(0), ALU.subtract, [tSM], [tSM])
                    K.stt(sm(0), sm(2), -1.0 / 1024.0, sm(0), ALU.mult, ALU.add, [tSM], [tSM])
                    K.ts(WTAB[0:nq, :], POW2[0:nq, :], sm(2), None, ALU.mult, rd=[tSM, tCST], wr=[tSM])
                    K.memset(CNT[0:nq, :], 0.0, wr=[tSM])
                    K.tt(sm(4), sm(0), WTAB[0:nq, 0:1], ALU.add, [tSM], [tSM])
                    for k in range(NITER):
                        K.ts(JUNK[0:nq, 0:L], Iv, sm(4), 0.0, ALU.is_ge, ALU.add, rd=[tSM] + tIr, wr=[tJ, tSM],
                             accum_out=CNT[0:nq, k:k + 1])
                        K.stt(sm(5), CNT[0:nq, k:k + 1], 255.5, WTAB[0:nq, k:k + 1], ALU.is_ge, ALU.mult, [tSM], [tSM])
                        K.stt(sm(4), sm(4), WTAB[0:nq, k + 1:k + 2], sm(5), ALU.subtract, ALU.add, [tSM], [tSM])
                    K.tt(sm(3), sm(4), WTAB[0:nq, NITER:NITER + 1], ALU.subtract, [tSM], [tSM])
                    K.stt(JUNK[0:nq, 0:L], Iv, sm(3), KPOS[0:nq, 0:L], ALU.is_ge, ALU.mult, [tSM, tKPOS] + tIr, [tJ])
                    K.red(sm(6), JUNK[0:nq, 0:L], ALU.max, [tJ], [tSM])
                    K.ts(MNEG[0:nq, 0:L], Iv, sm(3), -65536.0, ALU.is_lt, ALU.mult, rd=[tSM] + tIr, wr=[tMNEG])
                    K.ts(sm(7), sm(6), -1.0, T0T[0:nq, t0col:t0col + 1], ALU.mult, ALU.add, rd=[tSM, tCST], wr=[tSM])
                    K.ts(RTM[0:nq, :], MS[0:nq, :], sm(7), None, ALU.mult, rd=[tSM, tCST], wr=[tSM])
                    K.tt(DIAGX[0:nq, :, 0:nq], IDT16[0:nq, :, 0:nq],
                         RTM[0:nq, :].rearrange("p (h o) -> p h o", o=1).to_broadcast([nq, 16, nq]), ALU.mult, [tSM, tI16], [tDX])
                    if do_b:
                        select_b(nq, chunks, cx)

                def select_b(nq, chunks, cx):
                    MNEGT, tMT, ALR, tAR = cx['MNEGT'], cx['tMT'], cx['ALR'], cx['tAR']
                    for g in range(4):
                        pb, tp = fbank(0, 4)
                        K.mm(v3g(pb, 1, nq), ONESb[0:nq, 0:1], DIAGX[0:nq, 4 * g:4 * g + 4, 0:nq], True, True, [tONES, tDX], [tp])
                        K.copy(ALR[0:1, 4 * g:4 * g + 4, 0:nq], v3g(pb, 1, nq), [tp], [tAR])
                    for (ci, n_, k0) in chunks:
                        pbb, tpbb = bbank()
                        K.tr(pbb[0:n_, 0:nq], MNEG[0:nq, k0:k0 + n_], IDb[0:nq, 0:nq], [tMNEG, tIDb], [tpbb])
                        K.copy(MNEGT[0:n_, ci, 0:nq], pbb[0:n_, 0:nq], [tpbb], [tMT])

                def attn(nq, q0, chunks, ents, ot_dst, t_ot, cx=None, defer=False):
                    cx = cx or CTX[0]
                    QT, tQT, MNEGT, tMT, ALR, tAR = cx['QT'], cx['tQT'], cx['MNEGT'], cx['tMT'], cx['ALR'], cx['tAR']
                    acc, tacc, rs, trs = FB[4], tFB[4], FB[5], tFB[5]
                    v3 = lambda ap, rows: v3g(ap, rows, nq)
                    for g in range(4):
                        def score(idx):
                            ci, n_, k0 = chunks[idx]
                            sc, tsc = fbank(0, 4)
                            K.mm(v3(sc, n_), KT[:, g, k0:k0 + n_], QT[:, 4 * g:4 * g + 4, q0:q0 + nq], True, False, [tKT, tQT], [tsc])
                            K.mm(v3(sc, n_), ALIBL[0:5, ents[idx], 0:n_], ALR[0:5, 4 * g:4 * g + 4, 0:nq], False, False, [tAL, tAR], [tsc])
                            K.mm(v3(sc, n_), IDb[0:n_, 0:n_],
                                 MNEGT[0:n_, ci, 0:nq].rearrange("p (o q) -> p o q", o=1).to_broadcast([n_, 4, nq]),
                                 False, True, [tIDb, tMT], [tsc])
                            pt, tpt = PT[cnt["m"] % 2], tPT[cnt["m"] % 2]; cnt["m"] += 1
                            K.act(v3(pt, n_), v3(sc, n_), AF.Exp, [tsc], [tpt], bias=C0, scale=SCALE)
                            return pt, tpt
                        cur = score(0)
                        for idx, (ci, n_, k0) in enumerate(chunks):
                            last = idx == len(chunks) - 1
                            nxt = None if last else score(idx + 1)
                            pt, tpt = cur
                            K.mm(v3(acc, 128), VA[0:n_, ci, g * 128:(g + 1) * 128], v3(pt, n_), idx == 0, last, [tVA, tpt], [tacc])
                            K.mm(v3(rs, 128), ONESb[0:n_, :], v3(pt, n_), idx == 0, last, [tONES, tpt], [trs])
                            cur = nxt
                        if defer:
                            K.act(ACCS[:, g, :], acc[:, :], AF.Copy, [tacc], [tACCS])
                            K.act(RSS[:, g, :], rs[:, :], AF.Copy, [trs], [tACCS])
                        else:
                            K.op(DVE, lambda e, o=v3(RINV, 128), i_=v3(rs, 128): e.reciprocal(out=o, in_=i_), [trs], [tRINV])
                            K.tt(ot_dst[:, 4 * g:4 * g + 4, :], v3(acc, 128), v3(RINV, 128), ALU.mult, [tacc, tRINV], [t_ot])

                def finish_attn(ot_dst, t_ot):
                    K.op(DVE, lambda e: e.reciprocal(out=RSS[:, :, :], in_=RSS[:, :, :]), [tACCS], [tACCS])
                    K.tt(ot_dst.rearrange("d (g h) q -> d g (h q)", g=4), ACCS[:, :, :], RSS[:, :, :], ALU.mult, [tACCS], [t_ot])

                slot = {}

                def prep(i):
                    cx = CTX[i % 2]
                    K.dma(SP, cx['QT'][:], qTs[:, :, i * 128:(i + 1) * 128].rearrange("h d t -> d h t"), rd=[tqTs], wr=[cx['tQT']])
                    K.dma(SP, cx['IQT'][:], iqTs[:, :, i * 128:(i + 1) * 128].rearrange("h d t -> d h t"), rd=[tiqTs], wr=[cx['tIQT']])
                    nb = 4 * i + 4
                    chunks = [(0, 16, 0)] + [(1 + j, 128, 16 + 128 * j) for j in range(nb)]
                    ents = [i] + [8 + (j - 4 * i + 28) for j in range(nb)]
                    L = 16 + 128 * nb
                    tIr = [Tok() for _ in range(0, L, 512)]
                    indexer(128, 0, L, lambda h, i=i: WQ[:, i, h:h + 1], tIr, cx=cx)
                    select(128, chunks, L, CM[:, :], 512, i, tIr, cx=cx, do_b=False)
                    slot[i] = (chunks, ents)

                prep(0)
                select_b(128, slot[0][0], CTX[0])
                for i in range(8):
                    if i + 1 < 8:
                        prep(i + 1)
                    chunks, ents = slot[i]
                    attn(128, 0, chunks, ents, OTS[:, :, :], tOTS, cx=CTX[i % 2], defer=True)
                    if i + 1 < 8:
                        select_b(128, slot[i + 1][0], CTX[(i + 1) % 2])
                    finish_attn(OTS[:, :, :], tOTS)
                    K.dma(SP, oTs[:, :, i * 128:(i + 1) * 128].rearrange("h d t -> d h t"), OTS[:], rd=[tOTS], wr=[toTs])
                fence()
                stop("3p")

                K.dma(SP, QT[:], qTs[:, :, 1024:1152].rearrange("h d t -> d h t"), rd=[tqTs], wr=[tQT])
                K.dma(SP, IQT[:], iqTs[:, :, 1024:1152].rearrange("h d t -> d h t"), rd=[tiqTs], wr=[tIQT])
                K.memset(OTS[:], 0.0, wr=[tOTS])
                PTB = sc3("PTB", [128, 256], I32); IOTA = sc3("IOTA", [128, 1], I32); PIDX = sc3("PIDX", [128, 256], I32)
                tPIDX = Tok()
                K.dma(SP, PTB[:], ptab_d[0:1, :].partition_broadcast(128), wr=[tPIDX])
                K.dma(SP, IOTA[:], iota_d[:, :], wr=[tPIDX])
                K.ts(PIDX[:], PTB[:], 128, IOTA[:, 0:1], ALU.mult, ALU.add, rd=[tPIDX], wr=[tPIDX])
                WQ64 = sc3("WQ64", [64, 16], F32); SEL = sc3("SEL", [64, 16], F32); WQM = sc3("WQM", [64, 16, 16], F32)
                CMS64 = sc3("CMS64", [64, 4], F32)
                K.dma(SP, WQ64[:], wqs_d[:, :], rd=[twqsd], wr=[tWQS])
                K.dma(SP, SEL[:], sel_d[:, :], wr=[tWQS])
                K.dma(SP, CMS64[:], cms64_d[:, :], wr=[tCM])
                for s_i in range(16):
                    K.ts(WQM[:, s_i, :], WQ64[:, :], SEL[:, s_i:s_i + 1], None, ALU.mult, rd=[tWQS], wr=[tWQS])
                stop("3s0")
                IKGs = [ACCS[:, :, :].rearrange("p g (a b) -> p (g a) b", b=128), KT[:, 3, 2064:4112].rearrange("p (a b) -> p a b", b=128)]
                tIKG = [Tok(), Tok()]
                KGs = [VA[:, 17:25, :], VA[:, 25:33, :]]; tKG = [Tok(), Tok()]
                schunks = [(p, 128, 128 * p) for p in range(16)] + [(16, 4, 2048)]
                sents = [40 + p for p in range(16)] + [56]
                tKTc = [Tok() for _ in range(17)]; tVAc = [Tok() for _ in range(17)]

                def gather(dst_ap, tdst, src, col):
                    K.dma(POOL, None, None, rd=[tPIDX], wr=[tdst],
                          fn=lambda e, o=dst_ap, s_=src, c_=col: e.indirect_dma_start(
                              out=o, out_offset=None, in_=s_, in_offset=bass.IndirectOffsetOnAxis(ap=PIDX[:, c_:c_ + 1], axis=0)))

                LS = 2052
                tIs = [Tok() for _ in range(0, LS, 512)]
                for s_i in range(16):
                    ikg = IKGs[s_i % 2]
                    tik = tIKG[s_i % 2]
                    for p in range(16):
                        gather(ikg[:, p, 0:64], tik, cik_d[:, :], s_i * 16 + p)
                    K.copy(ikg[:, :, 64:128], ikg[:, :, 0:64], [tik], [tik])
                    for half in range(2):
                        pbb, tpbb = bbank()
                        for j in range(8):
                            K.tr(pbb[:, j * 128:(j + 1) * 128], ikg[:, half * 8 + j, :], IDb[:], [tik, tIDb], [tpbb], inc=(j == 7))
                        K.copy(IKT[:, half * 1024:(half + 1) * 1024], pbb[:, :], [tpbb], [tIKT])
                    c0 = 1040 + 4 * s_i
                    K.copy(IKT[:, 2048:2052], IKTO[:, c0:c0 + 4], [tIKTO], [tIKT])
                    indexer(64, 16, LS, lambda h, s_i=s_i: WQM[:, s_i, h:h + 1], tIs, first=(s_i == 0))
                stop("3s1")
                select(64, schunks, LS, CMS64[:, :], 4, 8, tIs)
                stop("3s2")
                acc, tacc, rs, trs = FB[4], tFB[4], FB[5], tFB[5]
                v16 = lambda ap, rows: ap[0:rows, 0:64].rearrange("p (h q) -> p h q", h=16)
                for s_i in range(int(os.environ.get("KNS3", "16"))):
                    qs = 16 + 4 * s_i
                    for half in range(2):
                        kg, tkg = KGs[half], tKG[half]
                        for p8 in range(8):
                            gather(kg[:, p8, :], tkg, ck_d[:, :], s_i * 16 + half * 8 + p8)
                    for p in range(16):
                        gather(VA[:, p, :], tVAc[p], cv_d[:, :], s_i * 16 + p)
                    K.dma(SP, VA[0:4, 16, :], vns[16 + 4 * s_i:20 + 4 * s_i, :], rd=[tagvi], wr=[tVAc[16]])
                    for p in range(16):
                        kg, tkg = KGs[p // 8], tKG[p // 8]
                        pbb, tpbb = bbank()
                        for g in range(4):
                            K.tr(pbb[:, g * 128:(g + 1) * 128], kg[:, p % 8, g * 128:(g + 1) * 128], IDb[:], [tkg, tIDb], [tpbb], inc=(g == 3))
                        K.copy(KT[:, :, 128 * p:128 * p + 128], pbb[:, 0:512].rearrange("d (g k) -> d g k", g=4), [tpbb], [tKTc[p]])
                    K.copy(KT[:, :, 2048:2052], KTO[:, :, qs + 1024:qs + 1028], [tKTO], [tKTc[16]])
                    def sscore(idx):
                        ci, n_, k0 = schunks[idx]
                        sc, tsc = fbank(0, 4)
                        K.mm(v16(sc, n_), ALIBL[0:5, sents[idx], 0:n_], ALR[0:5, 0:16, 4 * s_i:4 * s_i + 4], True, False, [tAL, tAR], [tsc])
                        for g in range(4):
                            K.mm(v16(sc, n_)[:, 4 * g:4 * g + 4, :], KT[:, g, k0:k0 + n_], QT[:, 4 * g:4 * g + 4, qs:qs + 4], False, False,
                                 [tKTc[ci], tQT], [tsc])
                        K.mm(v16(sc, n_), IDb[0:n_, 0:n_],
                             MNEGT[0:n_, ci, 4 * s_i:4 * s_i + 4].rearrange("p (o q) -> p o q", o=1).to_broadcast([n_, 16, 4]),
                             False, True, [tIDb, tMT], [tsc])
                        pt, tpt = PT[cnt["m"] % 2], tPT[cnt["m"] % 2]; cnt["m"] += 1
                        K.act(v16(pt, n_), v16(sc, n_), AF.Exp, [tsc], [tpt], bias=C0, scale=SCALE)
                        return pt, tpt
                    cur = sscore(0)
                    for idx, (ci, n_, k0) in enumerate(schunks):
                        last = idx == len(schunks) - 1
                        nxt = None if last else sscore(idx + 1)
                        pt, tpt = cur
                        cur = nxt
                        for g in range(4):
                            K.mm(v16(acc, 128)[:, 4 * g:4 * g + 4, :], VA[0:n_, ci, g * 128:(g + 1) * 128], v16(pt, n_)[:, 4 * g:4 * g + 4, :],
                                 idx == 0, last, [tVAc[ci], tpt], [tacc])
                        K.mm(v16(rs, 128), ONESb[0:n_, :], v16(pt, n_), idx == 0, last, [tONES, tpt], [trs])
                    K.op(DVE, lambda e, o=v16(RINV, 128), i_=v16(rs, 128): e.reciprocal(out=o, in_=i_), [trs], [tRINV])
                    K.tt(OTS[:, :, qs:qs + 4], v16(acc, 128), v16(RINV, 128), ALU.mult, [tacc, tRINV], [tOTS])
                K.dma(SP, oTs[:, :, 1024:1152].rearrange("h d t -> d h t"), OTS[:], rd=[tOTS], wr=[toTs])
            fence()
            stop("3")
            s23.close()
            mgTs = dscr("mgTs", [16, 128, T], BF16); tmgTs = Tok()
            s4a, sc4a = scoped()
            with s4a:
                XT = sc4a("XT4", [128, 16, T], BF16); tXT = Tok()
                OT = sc4a("OT", [128, 16, T], BF16); tOT = Tok()
                K.dma(SP, OT[:], oTs[:, :, :].rearrange("h d t -> d h t"), rd=[toTs], wr=[tOT])
                K.dma(SP, XT[:], XTs[:, :, :], rd=[tXTs], wr=[tXT])
                WA = [sc4a(f"WA{i}", [128, 16, 128], BF16) for i in range(4)]; tWA = [Tok() for _ in range(4)]
                SGT = [sc4a("SGU0", [128, 512], F32), sc4a("SGU1", [128, 512], F32)]; tSGT = [Tok(), Tok()]
                MA = [sc4a("MA0", [128, 512], F32), sc4a("MA1", [128, 512], F32)]; tMA = [Tok(), Tok()]
                MCC = [sc4a("MCD0", [128, T], BF16), sc4a("MCD1", [128, T], BF16)]; tMCC = [Tok(), Tok()]
                MGC = [sc4a("MGC0", [128, T], BF16), sc4a("MGC1", [128, T], BF16)]; tMGC = [Tok(), Tok()]
                n = 0
                for oc in range(16):
                    wa, twa = WA[(2 * oc) % 4], tWA[(2 * oc) % 4]
                    wg_, twg = WA[(2 * oc + 1) % 4], tWA[(2 * oc + 1) % 4]
                    K.dma(POOL, wa[:], wao[oc], wr=[twa]); K.dma(POOL, wg_[:], wfm[FM_GATTN + oc], wr=[twg])
                    mcc, tmcc = MCC[oc % 2], tMCC[oc % 2]
                    mgc, tmgc = MGC[oc % 2], tMGC[oc % 2]
                    K.dma(SP, mcc[:], mcTs[oc], rd=[tmcTs], wr=[tmcc])
                    for gi in range(3):
                        g0, gn = TG[gi]
                        pg, tpg = fbank()
                        for kc in range(16):
                            K.mm(pg[:, :gn], wg_[:, kc, :], XT[:, kc, g0:g0 + gn], kc == 0, kc == 15, [twg, tXT], [tpg])
                        py, tpy = fbank()
                        for kc in range(16):
                            K.mm(py[:, :gn], wa[:, kc, :], OT[:, kc, g0:g0 + gn], kc == 0, kc == 15, [twa, tOT], [tpy])
                        sg, tsg = SGT[n % 2], tSGT[n % 2]; ma, tma = MA[n % 2], tMA[n % 2]; n += 1
                        K.act(sg[:, :gn], pg[:, :gn], AF.Sigmoid, [tpg], [tsg])
                        K.tt(ma[:, :gn], sg[:, :gn], py[:, :gn], ALU.mult, [tsg, tpy], [tma])
                        K.tt(mgc[:, g0:g0 + gn], ma[:, :gn], mcc[:, g0:g0 + gn], ALU.add, [tma, tmcc], [tmgc])
                    K.dma(SP, mgTs[oc], mgc[:], rd=[tmgc], wr=[tmgTs])
            fence()
            s5, sc5 = scoped()
            with s5:
                XT = sc5("XT5", [128, 16, T], BF16); tXT = Tok()
                X = sc5("X2", [128, NT, D], F32)
                tX = [[Tok() for _ in range(4)] for _ in range(NT)]
                for t in range(NT):
                    K.dma(SP, X[:, t, :], X1s[t], rd=[tXs], wr=tX[t])
                    K.act(X[:, t, :], X[:, t, :], AF.Identity, tX[t], tX[t], scale=ALPHA)
                s5a, sc5a = scoped()
                with s5a:
                    MERGED = sc5a("MERGED", [128, 16, T], BF16); tMG = Tok()
                    K.dma(SP, MERGED[:], mgTs[:, :, :].rearrange("c p t -> p c t"), rd=[tmgTs], wr=[tMG])
                    WO = [sc5a("WO0", [128, 16, 256], BF16), sc5a("WO1", [128, 16, 256], BF16)]; tWO = [Tok(), Tok()]
                    for d8 in range(8):
                        wo_, two = WO[d8 % 2], tWO[d8 % 2]
                        K.dma(POOL, wo_[:], wo_d[d8], wr=[two])
                        for t in range(NT):
                            po, tpo = fbank()
                            for oc in range(16):
                                K.mm(po[:, 0:256], MERGED[:, oc, t * 128:(t + 1) * 128], wo_[:, oc, :], oc == 0, oc == 15, [tMG, two], [tpo])
                            xs = X[:, t, d8 * 256:(d8 + 1) * 256]
                            K.tt(xs, po[:, 0:256], xs, ALU.add, [tpo, tX[t][d8 // 2]], [tX[t][d8 // 2]])
                fence()
                sl, scl = scoped()
                with sl:
                    ln_tiles(X, tX, 1, scl)
                    to_fm(X, tX, XT, tXT, scl)
                fence()
                for t in range(NT):
                    K.act(X[:, t, :], X[:, t, :], AF.Identity, tX[t], tX[t], scale=ALPHA)
                sf, scf = scoped()
                with sf:
                    ffn(X, tX, XT, tXT, 1, scf)
                fence()
                sl, scl = scoped()
                with sl:
                    ln_tiles(X, tX, 2, scl)
                for t in range(NT):
                    K.dma(SP, y_blk[t] if t < 8 else y_misc[:, :], X[:, t, :], rd=tX[t], is_out=True)
            K.finish()
    except _Stop:
        pass
    return nc


_NC_CACHE = {}


def _bf16_round(x):
    x = np.asarray(x, dtype=np.float32)
    u = x.view(np.uint32).astype(np.uint64)
    u = ((u + 0x7FFF + ((u >> 16) & 1)) >> 16) << 16
    return u.astype(np.uint32).view(np.float32)


def kernel(x_prompt, x_sample, cache_k, cache_v, cache_idx_k, state_conv, page_table, meta_tokens,
           w_in, conv_w, w_conv_out, w_attn_out, w_o, ffn1_w_gu, ffn1_w_down, ffn2_w_gu, ffn2_w_down,
           ln1_g, ln1_b, ln2_g, ln2_b, ln3_g, ln3_b):
    f32 = np.float32
    A = lambda a: np.asarray(a)
    x_prompt, x_sample, meta_tokens = A(x_prompt), A(x_sample), A(meta_tokens)
    w_in0 = A(w_in)[0]
    def gu(w):
        return np.ascontiguousarray(A(w)[0].reshape(16, 128, 2, NU, 256).transpose(3, 1, 0, 2, 4))

    def dn(w):
        return np.ascontiguousarray(A(w)[0].reshape(NU, 2, 128, D).transpose(0, 2, 1, 3))

    def fmchunks(w):
        n = w.shape[1] // 128
        return np.ascontiguousarray(w.reshape(16, 128, n, 128).transpose(2, 1, 0, 3))

    cols = []
    for base, cnt in ((0, 16), (2048, 16), (4096, 16), (6144, 16), (8192, 4), (9216, 8)):
        for j in range(cnt):
            cols.append(np.arange(base + 128 * j, base + 128 * j + 128))
    cols.append(np.concatenate([np.arange(10240, 10304), np.arange(10240, 10304)]))
    for base in (10320, 12368):
        for j in range(16):
            cols.append(np.arange(base + 128 * j, base + 128 * j + 128))
    cols = np.concatenate(cols)
    wfm = fmchunks(w_in0[:, cols])
    tm = lambda w: np.ascontiguousarray(w.reshape(16, 128, w.shape[1]).transpose(1, 0, 2))
    shared = {
        "wgu1": gu(ffn1_w_gu), "wgu2": gu(ffn2_w_gu), "wd1": dn(ffn1_w_down), "wd2": dn(ffn2_w_down),
        "wfm": wfm, "wk": tm(w_in0[:, 8192:8704]), "wv": tm(w_in0[:, 8704:9216]), "wikw": tm(w_in0[:, 10240:10320]),
        "wco": fmchunks(A(w_conv_out)[0]), "wao": fmchunks(A(w_attn_out)[0]),
        "wo": np.ascontiguousarray(A(w_o)[0].reshape(16, 128, 8, 256).transpose(2, 1, 0, 3)),
        "lnp": np.stack([A(ln1_g)[0], A(ln1_b)[0], A(ln2_g)[0], A(ln2_b)[0], A(ln3_g)[0], A(ln3_b)[0]]).astype(f32),
        "convw": np.ascontiguousarray(A(conv_w)[0].reshape(3, 16, 128).transpose(2, 1, 0)),
        "ck": A(cache_k)[0][:NPOOL].reshape(NPOOL * 128, 512), "cv": A(cache_v)[0][:NPOOL].reshape(NPOOL * 128, 512),
        "cik": A(cache_idx_k)[0][:NPOOL].reshape(NPOOL * 128, 64),
        "iota": np.arange(128, dtype=np.int32).reshape(128, 1),
        "ident": np.eye(128, dtype=f32),
        "cms": np.where(np.arange(4)[None, :] <= np.arange(4)[:, None], 0.0, -1e30).astype(f32),
        "sel": np.repeat(np.eye(16, dtype=f32), 4, axis=0),
        "cms64": np.tile(np.where(np.arange(4)[None, :] <= np.arange(4)[:, None], 0.0, -1e30).astype(f32), (16, 1)),
        "pow2": (2.0 ** -(np.arange(NITER + 1) + 1.0)).astype(f32).reshape(1, NITER + 1),
        "kpos": np.arange(4112, dtype=f32).reshape(1, 4112),
        "idt16": np.ascontiguousarray(np.broadcast_to(np.eye(128, dtype=f32)[:, None, :], (128, 16, 128))),
    }
    slopes = 2.0 ** (-(np.arange(16) + 1.0) / 2.0)
    msv = (slopes * np.sqrt(128.0)).astype(f32)
    hi = _bf16_round(msv)
    lo = _bf16_round(msv - hi)
    shared["ms"] = msv.reshape(1, 16)
    shared["alibr"] = np.stack([np.repeat(128 * hi, 128), np.repeat(hi, 128), np.repeat(128 * lo, 128),
                                np.repeat(lo, 128)]).astype(f32)
    pt = A(page_table).astype(np.int32) % NPOOL
    sc_all = A(state_conv)[0]
    in_maps = []
    p_ = np.arange(128)
    for core in range(8):
        g, c = core // 4, core % 4
        xin = np.zeros((NT, 128, D), f32)
        for i in range(8):
            j = 4 * i + c
            xin[i] = x_prompt[g, 128 * j:128 * j + 128]
            if j > 0:
                xin[8, 80 + 2 * i:82 + 2 * i] = x_prompt[g, 128 * j - 2:128 * j]
            else:
                xin[8, 80 + 2 * i:82 + 2 * i] = meta_tokens[14:16]
        xin[8, 0:16] = meta_tokens
        xin[8, 16:80] = x_sample[16 * core:16 * core + 16].reshape(64, D)
        xT = np.ascontiguousarray(xin.reshape(T, D).T.reshape(16, 128, T).transpose(1, 0, 2))
        cm = np.full((128, 4, 128), -1e30, f32)
        for dl in range(4):
            if dl < c:
                cm[:, dl, :] = 0.0
            elif dl == c:
                cm[:, dl, :] = np.where(p_[None, :] <= p_[:, None], 0.0, -1e30)
        al = np.zeros((5, 57, 128), f32)
        al[0] = 1.0
        for e in range(57):
            if e < 8:
                a_, b_ = -(4 * e + c + 1), np.where(p_ < 16, p_ - 15, 0)
            elif e < 40:
                a_, b_ = (e - 8 - 28) - c, p_ - 127
            elif e < 56:
                a_, b_ = (e - 40) - 16, p_ - 3
            else:
                a_, b_ = 0, np.where(p_ < 4, p_ - 3, 0)
            al[1, e] = a_; al[3, e] = a_; al[2, e] = b_; al[4, e] = b_
        t0 = np.array([143 + 128 * (4 * i + c) for i in range(8)] + [2051], f32).reshape(1, 9)
        sc = sc_all[16 * core:16 * core + 16]
        m = dict(shared)
        m.update({
            "xin": xin, "xTin": xT, "cmask": cm.reshape(128, 512), "alibl": al, "t0tab": t0,
            "sconv": np.ascontiguousarray(sc.reshape(16, 2, 16, 128).transpose(3, 2, 0, 1)),
            "ptab": np.ascontiguousarray(pt[16 * core:16 * core + 16].reshape(1, 256)),
        })
        in_maps.append(m)
    if "nc" not in _NC_CACHE:
        _NC_CACHE["nc"] = build_program()
    res = run_bass_kernel_spmd(_NC_CACHE["nc"], in_maps[:KCORES], core_ids=list(range(KCORES)))
    R = [{k: np.asarray(v) for k, v in r.items()} for r in res.results]
    B, S = 2, 4096
    y_prompt = np.zeros((B, S, D), f32); y_sample = np.zeros((128, 4, D), f32)
    nkp = np.zeros((1, B, S + 16, 4, 128), f32); nvp = np.zeros_like(nkp); nip = np.zeros((1, B, S + 16, 64), f32)
    ncp = np.zeros((1, B, 2, D), f32)
    nks = np.zeros((1, 128, 4, 4, 128), f32); nvs = np.zeros_like(nks); nis = np.zeros((1, 128, 4, 64), f32)
    ncs = np.zeros((1, 128, 2, D), f32)
    for core in range(KCORES):
        g, c = core // 4, core % 4
        r = R[core]
        for i in range(8):
            j = 4 * i + c
            y_prompt[g, 128 * j:128 * j + 128] = r["y_blk"][i]
            nkp[0, g, 16 + 128 * j:144 + 128 * j] = r["k_blk"][i].reshape(128, 4, 128)
            nvp[0, g, 16 + 128 * j:144 + 128 * j] = r["v_blk"][i].reshape(128, 4, 128)
            nip[0, g, 16 + 128 * j:144 + 128 * j] = r["ik_blk"][i]
        if c == 0:
            nkp[0, g, 0:16] = r["k_misc"][0:16].reshape(16, 4, 128)
            nvp[0, g, 0:16] = r["v_misc"][0:16].reshape(16, 4, 128)
            nip[0, g, 0:16] = r["ik_misc"][0:16]
        if c == 3:
            ncp[0, g] = r["ustate"][32:34]
        sl = slice(16 * core, 16 * core + 16)
        y_sample[sl] = r["y_misc"][16:80].reshape(16, 4, D)
        nks[0, sl] = r["k_misc"][16:80].reshape(16, 4, 4, 128)
        nvs[0, sl] = r["v_misc"][16:80].reshape(16, 4, 4, 128)
        nis[0, sl] = r["ik_misc"][16:80].reshape(16, 4, 64)
        ncs[0, sl] = r["ustate"][0:32].reshape(16, 2, D)
    return (y_prompt, y_sample, nkp, nvp, nip, ncp, nks, nvs, nis, ncs)
```
